# Optimizing a Trainium2 kernel written in Bass

```python
import math
import jax, jax.numpy as jnp
from jax import lax
import numpy as np

D_MODEL = 1024
BATCH = 4
SEQ = 8192
DEPTH = 1

GRID_W = 64
CTX_LEN = 256
HEAD_DIM = 64
N_HEADS_DIFF = D_MODEL // (4 * HEAD_DIM)
N_HEADS_NA = D_MODEL // (2 * HEAD_DIM)
DIFF_WIDTH = N_HEADS_DIFF * 2 * HEAD_DIM
NA_WIDTH = N_HEADS_NA * HEAD_DIM
MIX_WIDTH = DIFF_WIDTH + NA_WIDTH
NA_ROWS_MAX = 8
NA_COLS = 16
ROPE_BASE = 10000.0
ROPE_FREQS = HEAD_DIM // 4
Q_BLOCK = 128
N_EXPERTS = 32
TOP_K = 4
D_FF = D_MODEL
SWIGLU_ALPHA = 1.702
SWIGLU_LIMIT = 7.0
EXPERT_BLOCK = 256
NORM_EPS = 1e-6

kernel_name = 'hybrid_diffattn_natten_moe_dit_layer'


def rms_norm(x, g):
    xf = x.astype(jnp.float32)
    y = xf * lax.rsqrt(jnp.mean(xf * xf, axis=-1, keepdims=True) + NORM_EPS)
    return (y * g.astype(jnp.float32)).astype(x.dtype)


def modulate(h, shift, scale):
    return h * (1.0 + scale) + shift


def adaln(cv, w_ada, b_ada):
    return (jax.nn.silu(cv) @ w_ada + b_ada).reshape(cv.shape[0], 6, -1)


def axial_rope_tables(row, col):
    inv = 1.0 / (ROPE_BASE ** (jnp.arange(ROPE_FREQS, dtype=jnp.float32) / ROPE_FREQS))
    ang_r = row.astype(jnp.float32)[:, None] * inv
    ang_c = col.astype(jnp.float32)[:, None] * inv
    return (jnp.cos(ang_r), jnp.sin(ang_r), jnp.cos(ang_c), jnp.sin(ang_c))


def _rotate(x, cos, sin):
    x1, x2 = x[..., :ROPE_FREQS], x[..., ROPE_FREQS:]
    return jnp.concatenate([x1 * cos - x2 * sin, x2 * cos + x1 * sin], axis=-1)


def apply_axial_rope(x, tables):
    cr, sr, cc, sc = [t.reshape((t.shape[0],) + (1,) * (x.ndim - 3) + (ROPE_FREQS,)) for t in tables]
    half = HEAD_DIM // 2
    out = jnp.concatenate([_rotate(x[..., :half], cr, sr), _rotate(x[..., half:], cc, sc)], axis=-1)
    return out.astype(x.dtype)


def project_heads(h, w_in, qn_d, kn_d, qn_n, kn_n):
    p = h @ w_in
    bn, n = p.shape[:2]
    cuts = [DIFF_WIDTH, 2 * DIFF_WIDTH, 3 * DIFF_WIDTH, 3 * DIFF_WIDTH + NA_WIDTH, 3 * DIFF_WIDTH + 2 * NA_WIDTH]
    qd, kd, vd, qn, kn, vn = jnp.split(p, cuts, axis=-1)
    qd = rms_norm(qd.reshape(bn, n, N_HEADS_DIFF, 2, HEAD_DIM), qn_d)
    kd = rms_norm(kd.reshape(bn, n, N_HEADS_DIFF, 2, HEAD_DIM), kn_d)
    vd = vd.reshape(bn, n, N_HEADS_DIFF, 2 * HEAD_DIM)
    qn = rms_norm(qn.reshape(bn, n, N_HEADS_NA, HEAD_DIM), qn_n)
    kn = rms_norm(kn.reshape(bn, n, N_HEADS_NA, HEAD_DIM), kn_n)
    vn = vn.reshape(bn, n, N_HEADS_NA, HEAD_DIM)
    return qd, kd, vd, qn, kn, vn


def diff_attend(q, k, v, lam):
    s = jnp.einsum('bqhmd,bkhmd->bhmqk', q, k).astype(jnp.float32) * (HEAD_DIM ** -0.5)
    p = jax.nn.softmax(s, axis=-1).astype(v.dtype)
    o = jnp.einsum('bhmqk,bkhe->bqhme', p, v)
    return o[:, :, :, 0] - lam.astype(o.dtype) * o[:, :, :, 1]


def diff_attention_blocks(q, k, v, lam):
    b, s = q.shape[:2]
    nb = s // Q_BLOCK
    qb = q.reshape(b, nb, Q_BLOCK, N_HEADS_DIFF, 2, HEAD_DIM).swapaxes(0, 1)
    out = lax.map(lambda qq: diff_attend(qq, k, v, lam), qb)
    return out.swapaxes(0, 1).reshape(b, s, N_HEADS_DIFF, 2 * HEAD_DIM)


def softmax_attention(q, k, v):
    s = jnp.einsum('bqhd,bkhd->bhqk', q, k).astype(jnp.float32) * (HEAD_DIM ** -0.5)
    p = jax.nn.softmax(s, axis=-1).astype(v.dtype)
    return jnp.einsum('bhqk,bkhd->bqhd', p, v)


def neighbourhood_attention(q, k, v, k_ctx, v_ctx, rpb, rows_n):
    b, s, h, dh = q.shape
    kr = min(NA_ROWS_MAX, rows_n)
    kc = NA_COLS
    qg = q.reshape(b, rows_n, GRID_W, h, dh).swapaxes(0, 1)
    kg = k.reshape(b, rows_n, GRID_W, h, dh)
    vg = v.reshape(b, rows_n, GRID_W, h, dh)
    cols = jnp.arange(GRID_W)
    col_start = jnp.clip(cols - kc // 2, 0, GRID_W - kc)
    col_idx = col_start[:, None] + jnp.arange(kc)[None, :]
    col_bias_idx = col_idx - cols[:, None] + (NA_COLS - 1)
    scale = HEAD_DIM ** -0.5

    def row_block(args):
        r, q_r = args
        r0 = jnp.clip(r - kr // 2, 0, rows_n - kr)
        k_rows = lax.dynamic_slice_in_dim(kg, r0, kr, axis=1)
        v_rows = lax.dynamic_slice_in_dim(vg, r0, kr, axis=1)
        k_win = k_rows[:, :, col_idx]
        v_win = v_rows[:, :, col_idx]
        row_bias_idx = r0 + jnp.arange(kr) - r + (NA_ROWS_MAX - 1)
        bias = rpb[:, row_bias_idx][:, :, col_bias_idx]
        s_lat = jnp.einsum('bwhd,brwkhd->bhwrk', q_r, k_win).astype(jnp.float32) * scale
        s_lat = s_lat + bias.transpose(0, 2, 1, 3)[None].astype(jnp.float32)
        s_ctx = jnp.einsum('bwhd,bchd->bhwc', q_r, k_ctx).astype(jnp.float32) * scale
        sc = jnp.concatenate([s_lat.reshape(b, h, GRID_W, kr * kc), s_ctx], axis=-1)
        p = jax.nn.softmax(sc, axis=-1).astype(v.dtype)
        p_lat = p[..., :kr * kc].reshape(b, h, GRID_W, kr, kc)
        p_ctx = p[..., kr * kc:]
        return (jnp.einsum('bhwrk,brwkhd->bwhd', p_lat, v_win)
                + jnp.einsum('bhwc,bchd->bwhd', p_ctx, v_ctx))

    out = lax.map(row_block, (jnp.arange(rows_n), qg))
    return out.swapaxes(0, 1).reshape(b, s, h, dh)


def routed_swiglu_moe(h, w_router, b_router, w_gate_up, b_gate_up, w_down, b_down):
    t, d = h.shape
    logits = (h @ w_router + b_router).astype(jnp.float32)
    top_vals, top_idx = lax.top_k(logits, TOP_K)
    gates = jax.nn.softmax(top_vals, axis=-1)
    n_assign = t * TOP_K
    e_flat = top_idx.reshape(-1)
    order = jnp.argsort(e_flat)
    e_sorted = e_flat[order]
    tok_sorted = order // TOP_K
    gate_sorted = gates.reshape(-1)[order]
    counts = jnp.bincount(e_flat, length=N_EXPERTS)
    starts = jnp.cumsum(counts) - counts
    padded = (counts + EXPERT_BLOCK - 1) // EXPERT_BLOCK * EXPERT_BLOCK
    pad_ends = jnp.cumsum(padded)
    pad_starts = pad_ends - padded
    dest = pad_starts[e_sorted] + jnp.arange(n_assign) - starts[e_sorted]
    n_blocks = -(-n_assign // EXPERT_BLOCK) + N_EXPERTS
    buf = jnp.zeros((n_blocks * EXPERT_BLOCK, d), h.dtype).at[dest].set(h[tok_sorted])
    block_expert = jnp.minimum(
        jnp.searchsorted(pad_ends, jnp.arange(n_blocks) * EXPERT_BLOCK, side='right'), N_EXPERTS - 1)

    def expert_block(args):
        xb, e = args
        gu = xb @ w_gate_up[e] + b_gate_up[e]
        glu = jnp.minimum(gu[:, :D_FF], SWIGLU_LIMIT)
        lin = jnp.clip(gu[:, D_FF:], -SWIGLU_LIMIT, SWIGLU_LIMIT)
        act = glu * jax.nn.sigmoid(SWIGLU_ALPHA * glu) * (lin + 1.0)
        return act @ w_down[e] + b_down[e]

    ys = lax.map(expert_block, (buf.reshape(n_blocks, EXPERT_BLOCK, d), block_expert)).reshape(-1, d)
    contrib = ys[dest] * gate_sorted[:, None].astype(ys.dtype)
    return jax.ops.segment_sum(contrib, tok_sorted, num_segments=t)


def hybrid_layer(x, ctx, c, c_ctx, lp, layer_idx, rope, rows_n, update_ctx):
    b, s, d = x.shape
    mod = adaln(c, lp['w_ada'], lp['b_ada'])
    mod_c = adaln(c_ctx[None, :], lp['w_ada'], lp['b_ada'])
    sh1, sc1, g1, sh2, sc2, g2 = [mod[:, i][:, None, :] for i in range(6)]
    sh1c, sc1c, g1c, sh2c, sc2c, g2c = [mod_c[:, i][:, None, :] for i in range(6)]

    h_lat = modulate(rms_norm(x, lp['g_attn']), sh1, sc1)
    h_ctx = modulate(rms_norm(ctx, lp['g_attn']), sh1c, sc1c)
    norms = (lp['q_norm_diff'], lp['k_norm_diff'], lp['q_norm_na'], lp['k_norm_na'])
    qd, kd, vd, qn, kn, vn = project_heads(h_lat, lp['w_in'], *norms)
    qd_c, kd_c, vd_c, qn_c, kn_c, vn_c = project_heads(h_ctx, lp['w_in'], *norms)
    qd = apply_axial_rope(qd, rope)
    kd = apply_axial_rope(kd, rope)

    lam_init = 0.8 - 0.6 * math.exp(-0.3 * layer_idx)
    lam = (jnp.exp(jnp.sum(lp['lam_q1'].astype(jnp.float32) * lp['lam_k1'].astype(jnp.float32)))
           - jnp.exp(jnp.sum(lp['lam_q2'].astype(jnp.float32) * lp['lam_k2'].astype(jnp.float32)))
           + lam_init)

    kd_all = jnp.concatenate([kd, kd_c], axis=1)
    vd_all = jnp.concatenate([vd, vd_c], axis=1)
    o_diff = rms_norm(diff_attention_blocks(qd, kd_all, vd_all, lam), lp['subln_diff']) * (1.0 - lam_init)
    o_na = neighbourhood_attention(qn, kn, vn, kn_c, vn_c, lp['rpb_na'], rows_n)
    o_na = rms_norm(o_na, lp['out_norm_na'].reshape(N_HEADS_NA, HEAD_DIM))
    mix = jnp.concatenate([o_diff.reshape(b, s, DIFF_WIDTH), o_na.reshape(b, s, NA_WIDTH)], axis=-1)
    x = x + g1 * (mix @ lp['w_out'])

    moe_w = (lp['w_router'], lp['b_router'], lp['w_gate_up'], lp['b_gate_up'], lp['w_down'], lp['b_down'])
    h2 = modulate(rms_norm(x, lp['g_ffn']), sh2, sc2)
    x = x + g2 * routed_swiglu_moe(h2.reshape(-1, d), *moe_w).reshape(b, s, d)

    if update_ctx:
        oc_diff = rms_norm(diff_attend(qd_c, kd_c, vd_c, lam), lp['subln_diff']) * (1.0 - lam_init)
        oc_na = rms_norm(softmax_attention(qn_c, kn_c, vn_c), lp['out_norm_na'].reshape(N_HEADS_NA, HEAD_DIM))
        bc, nc = ctx.shape[:2]
        mix_c = jnp.concatenate([oc_diff.reshape(bc, nc, DIFF_WIDTH), oc_na.reshape(bc, nc, NA_WIDTH)], axis=-1)
        ctx = ctx + g1c * (mix_c @ lp['w_out'])
        h2c = modulate(rms_norm(ctx, lp['g_ffn']), sh2c, sc2c)
        ctx = ctx + g2c * routed_swiglu_moe(h2c.reshape(-1, d), *moe_w).reshape(ctx.shape)
    return x, ctx


def setup_inputs(seed: int = 0) -> dict:
    key = jax.random.key(seed)
    ks = jax.random.split(key, 32)
    f32 = jnp.float32
    nrm = lambda k, shape, sc: jax.random.normal(k, shape, f32) * sc
    L, D, E, F = DEPTH, D_MODEL, N_EXPERTS, D_FF
    return {
        'x': nrm(ks[0], (BATCH, SEQ, D), 1.0),
        'c': nrm(ks[1], (BATCH, D), 1.0),
        'ctx': nrm(ks[2], (BATCH, CTX_LEN, D), 1.0),
        'c_ctx': nrm(ks[3], (D,), 1.0),
        'w_ada': nrm(ks[4], (L, D, 6 * D), 0.5 * D ** -0.5),
        'b_ada': nrm(ks[5], (L, 6 * D), 0.01),
        'g_attn': 1.0 + nrm(ks[6], (L, D), 0.05),
        'w_in': nrm(ks[7], (L, D, 3 * MIX_WIDTH), D ** -0.5),
        'q_norm_diff': 1.0 + nrm(ks[8], (L, HEAD_DIM), 0.05),
        'k_norm_diff': 1.0 + nrm(ks[9], (L, HEAD_DIM), 0.05),
        'lam_q1': nrm(ks[10], (L, HEAD_DIM), 0.1),
        'lam_k1': nrm(ks[11], (L, HEAD_DIM), 0.1),
        'lam_q2': nrm(ks[12], (L, HEAD_DIM), 0.1),
        'lam_k2': nrm(ks[13], (L, HEAD_DIM), 0.1),
        'subln_diff': 1.0 + nrm(ks[14], (L, 2 * HEAD_DIM), 0.05),
        'q_norm_na': 1.0 + nrm(ks[15], (L, HEAD_DIM), 0.05),
        'k_norm_na': 1.0 + nrm(ks[16], (L, HEAD_DIM), 0.05),
        'rpb_na': nrm(ks[17], (L, N_HEADS_NA, 2 * NA_ROWS_MAX - 1, 2 * NA_COLS - 1), 0.1),
        'out_norm_na': 1.0 + nrm(ks[18], (L, NA_WIDTH), 0.05),
        'w_out': nrm(ks[19], (L, MIX_WIDTH, D), MIX_WIDTH ** -0.5),
        'g_ffn': 1.0 + nrm(ks[20], (L, D), 0.05),
        'w_router': nrm(ks[21], (L, D, E), D ** -0.5),
        'b_router': nrm(ks[22], (L, E), 0.01),
        'w_gate_up': nrm(ks[23], (L, E, D, 2 * F), D ** -0.5),
        'b_gate_up': nrm(ks[24], (L, E, 2 * F), 0.01),
        'w_down': nrm(ks[25], (L, E, F, D), F ** -0.5),
        'b_down': nrm(ks[26], (L, E, D), 0.01),
    }


def reference(x, c, ctx, c_ctx, w_ada, b_ada, g_attn, w_in, q_norm_diff, k_norm_diff,
              lam_q1, lam_k1, lam_q2, lam_k2, subln_diff, q_norm_na, k_norm_na, rpb_na,
              out_norm_na, w_out, g_ffn, w_router, b_router, w_gate_up, b_gate_up, w_down, b_down):
    s = x.shape[1]
    rows_n = s // GRID_W
    pos = jnp.arange(s, dtype=jnp.int32)
    rope = axial_rope_tables(pos // GRID_W, pos % GRID_W)
    for l in range(DEPTH):
        lp = {
            'w_ada': w_ada[l], 'b_ada': b_ada[l], 'g_attn': g_attn[l], 'w_in': w_in[l],
            'q_norm_diff': q_norm_diff[l], 'k_norm_diff': k_norm_diff[l],
            'lam_q1': lam_q1[l], 'lam_k1': lam_k1[l], 'lam_q2': lam_q2[l], 'lam_k2': lam_k2[l],
            'subln_diff': subln_diff[l], 'q_norm_na': q_norm_na[l], 'k_norm_na': k_norm_na[l],
            'rpb_na': rpb_na[l], 'out_norm_na': out_norm_na[l], 'w_out': w_out[l], 'g_ffn': g_ffn[l],
            'w_router': w_router[l], 'b_router': b_router[l], 'w_gate_up': w_gate_up[l],
            'b_gate_up': b_gate_up[l], 'w_down': w_down[l], 'b_down': b_down[l],
        }
        x, ctx = hybrid_layer(x, ctx, c, c_ctx, lp, l, rope, rows_n, l < DEPTH - 1)
    return x
```

```python
import numpy as np
from contextlib import ExitStack
import concourse.bass as bass
import concourse.mybir as mybir
from concourse.bass_utils import run_bass_kernel_spmd

F32 = mybir.dt.float32
BF16 = mybir.dt.bfloat16
I32 = mybir.dt.int32
AF = mybir.ActivationFunctionType
ALU = mybir.AluOpType
AX = mybir.AxisListType

NCORES = 8
D = 1024
NT_OWN = 32
NT_KEY = 66
NT_ALL = 70
NKEY = NT_KEY * 128
NQ = NT_OWN * 128
BS = 512
NBLK = 64
NSLOT = NBLK * BS
MASKV = -30000.0
EPS = 1e-6
LAM_INIT = 0.8 - 0.6 * 1.0


class Tk:
    __slots__ = ("sem", "val", "sid")

    def __init__(self, sem, val, sid):
        self.sem, self.val, self.sid = sem, val, sid


class Res:
    __slots__ = ("w", "r", "multi")

    def __init__(self, multi=False):
        self.w = {}
        self.r = {}
        self.multi = multi


class Q:
    LIMIT = 12000

    def __init__(self, K, name, eng):
        self.K, self.name, self.eng = K, name, eng
        self.sem, self.sid = K.new_sem(name)
        self.count = 0
        self.waited = {}

    def wait(self, tk):
        if tk is None or self.waited.get(tk.sid, 0) >= tk.val:
            return
        self.eng.wait_ge(tk.sem, tk.val)
        self.waited[tk.sid] = tk.val

    def _rot(self):
        if self.count >= self.LIMIT:
            self.sem, self.sid = self.K.new_sem(self.name)
            self.count = 0

    def mark(self, instr):
        self._rot()
        self.count += 1
        instr.then_inc(self.sem, 1)
        return Tk(self.sem, self.count, self.sid)

    def future(self):
        self._rot()
        return Tk(self.sem, self.count + 1, self.sid)

    def last(self):
        return Tk(self.sem, self.count, self.sid) if self.count else None


class DmaStream:
    LIMIT = 700

    def __init__(self, K, name):
        self.K, self.name = K, name
        self.sem, self.sid = K.new_sem(name)
        self.n = 0

    def mark(self, instr):
        if self.n >= self.LIMIT:
            self.sem, self.sid = self.K.new_sem(self.name)
            self.n = 0
        self.n += 1
        instr.then_inc(self.sem, 16)
        return Tk(self.sem, 16 * self.n, self.sid)

    def last(self):
        return Tk(self.sem, 16 * self.n, self.sid) if self.n else None


class Kn:
    def __init__(self, nc):
        self.nc = nc
        self.es = ExitStack()
        self.nsem = 0
        self.pe = Q(self, "pe", nc.tensor)
        self.act = Q(self, "act", nc.scalar)
        self.dve = Q(self, "dve", nc.vector)
        self.pool = Q(self, "pool", nc.gpsimd)
        self.sp = Q(self, "sp", nc.sync)
        self.queues = [self.pe, self.act, self.dve, self.pool, self.sp]
        self.streams = {}
        self.old_streams = []

    def new_sem(self, name):
        self.nsem += 1
        s = self.es.enter_context(self.nc.semaphore(f"s_{name}_{self.nsem}"))
        return s, self.nsem

    def stream(self, name):
        if name not in self.streams:
            self.streams[name] = DmaStream(self, name)
        return self.streams[name]

    def op(self, q, fn, reads=(), writes=(), mark=True):
        for r in reads:
            for t in r.w.values():
                q.wait(t)
        for w in writes:
            for sid, t in w.w.items():
                if sid != q.sid:
                    q.wait(t)
            for sid, t in w.r.items():
                if sid != q.sid:
                    q.wait(t)
        instr = fn()
        tk = q.mark(instr) if mark else q.future()
        for r in reads:
            r.r[tk.sid] = tk
        for w in writes:
            w.w = {tk.sid: tk}
            w.r = {}
        return tk

    def dma(self, q, stream, fn, reads=(), writes=()):
        st = self.stream(stream)
        for r in reads:
            for t in r.w.values():
                q.wait(t)
        for w in writes:
            if w.multi:
                continue
            for t in w.w.values():
                if t.sid != st.sid:
                    q.wait(t)
            for t in w.r.values():
                q.wait(t)
        instr = fn()
        tk = st.mark(instr)
        for r in reads:
            r.r[tk.sid] = tk
        for w in writes:
            if w.multi:
                w.w[tk.sid] = tk
            else:
                w.w = {tk.sid: tk}
                w.r = {}
        return tk

    def barrier(self, full=False):
        tks = [q.last() for q in self.queues] + [s.last() for n, s in self.streams.items()
                                                  if full or not n.startswith("pc")]
        for q in self.queues:
            for t in tks:
                if t is not None and t.sid != q.sid:
                    q.wait(t)


def build():
    nc = bass.Bass("TRN2", target_bir_lowering=False)
    k = Kn(nc)

    def din(name, shape, dt=F32):
        return nc.dram_tensor(name, shape, dt, kind="ExternalInput").ap()

    def dscr(name, shape, dt):
        return nc.dram_tensor(name, shape, dt, kind="Internal").ap()

    x_all = din("x_all", [NT_ALL * 128, D])
    rope = din("rope", [64 * 128, 128])
    cT_d = din("cT", [128, 16])
    badaT_d = din("badaT", [128, 48])
    gT_d = din("gT", [128, 16])
    gains_d = din("gains", [1, 5 * 512])
    lamv_d = din("lamv", [1, 256])
    subln_d = din("sublnT", [128, 1])
    biasm_d = din("biasm", [5, 128, 6 * 8 * 128])
    w_ada_d = din("w_ada", [D, 6 * D])
    w_in_d = din("w_in", [D, 3 * D])
    w_out_d = din("w_out", [D, D])
    wr_d = din("w_routerT", [128, 8 * 32])
    br_d = din("b_router", [1, 32])
    wgu_d = din("w_gate_up", [32 * D, 2 * D])
    wd_d = din("w_down", [32 * D, D])
    bgu_d = din("b_gate_up", [32 * 16, 128])
    bd_d = din("b_down", [32, D])
    bstart_d = din("bstart", [1, NBLK])
    basepk_d = din("basepk", [128, 9])
    out_d = nc.dram_tensor("out", [NQ, D], F32, kind="ExternalOutput").ap()

    kdt_s = dscr("kdt_s", [4, 128, NKEY], BF16)
    qdt_s = dscr("qdt_s", [4, 128, NQ], BF16)
    vd_s = dscr("vd_s", [4, 128, NT_KEY * 128], BF16)
    buf_s = dscr("buf_s", [NSLOT, D], BF16)
    ys_s = dscr("ys_s", [NSLOT, D], F32)
    qnt_s = dscr("qnt_s", [4, 128, NQ], BF16)
    mixn_s = dscr("mixn_s", [NQ, 512], BF16)
    mixT_s = dscr("mixT_s", [4, 128, NQ], BF16)
    h2_s = dscr("h2_s", [NQ, D], BF16)
    wgu_b = dscr("wgu_b", [32 * D, 2 * D], BF16)
    wd_b = dscr("wd_b", [32 * D, D], BF16)
    wb_r = Res(True)
    kdt_r, qdt_r, vd_r, buf_r, ys_r = Res(True), Res(True), Res(True), Res(True), Res(True)
    qnt_r, mixn_r, mixd_r, h2s_r = Res(True), Res(True), Res(True), Res(True)
    out_r = [Res() for _ in range(NT_OWN)]

    gl = k.es

    def sb(es, name, shape, dt):
        return es.enter_context(nc.sbuf_tensor('sb_' + name, shape, dt))

    def ps(es, name, shape, dt):
        return es.enter_context(nc.psum_tensor('pp_' + name, shape, dt))

    V, A, P, T, SP = k.dve, k.act, k.pool, k.pe, k.sp
    v_, a_, g_, t_ = nc.vector, nc.scalar, nc.gpsimd, nc.tensor

    ident_b = sb(gl, "ident_b", [128, 128], BF16)
    ident_f = sb(gl, "ident_f", [128, 128], F32)
    ones_b = sb(gl, "ones_b", [128, 512], BF16)
    ones_f = sb(gl, "ones_f", [128, 128], F32)
    U_b = sb(gl, "U_b", [128, 128], BF16)
    epsc = sb(gl, "epsc", [128, 1], F32)
    cst_r = Res()
    k.op(P, lambda: g_.memset(ident_f[:], 0.0), writes=[cst_r])
    k.op(P, lambda: g_.affine_select(out=ident_f[:], in_=ident_f[:], pattern=[[-1, 128]], compare_op=ALU.not_equal,
                                     fill=1.0, base=0, channel_multiplier=1), reads=[cst_r], writes=[cst_r])
    k.op(P, lambda: g_.tensor_copy(out=ident_b[:], in_=ident_f[:]), reads=[cst_r], writes=[cst_r])
    k.op(P, lambda: g_.memset(ones_b[:], 1.0), writes=[cst_r])
    k.op(P, lambda: g_.memset(ones_f[:], 1.0), writes=[cst_r])
    k.op(P, lambda: g_.memset(U_b[:], 1.0), writes=[cst_r])
    k.op(P, lambda: g_.affine_select(out=U_b[:], in_=U_b[:], pattern=[[1, 128]], compare_op=ALU.is_gt,
                                     fill=0.0, base=0, channel_multiplier=-1), reads=[cst_r], writes=[cst_r])
    k.op(P, lambda: g_.memset(epsc[:], EPS), writes=[cst_r])

    cT = sb(gl, "cT", [128, 16], F32)
    scT = sb(gl, "scT", [128, 16], F32)
    badaT = sb(gl, "badaT", [128, 48], F32)
    gT = sb(gl, "gT", [128, 16], F32)
    gains = sb(gl, "gains", [128, 5 * 512], F32)
    lamv = sb(gl, "lamv", [128, 256], F32)
    sublnc = sb(gl, "sublnc", [128, 1], F32)
    brt = sb(gl, "brt", [128, 32], F32)
    bstart = sb(gl, "bstart", [128, NBLK], F32)
    basepk = sb(gl, "basepk", [128, 9], F32)
    wr_sb = sb(gl, "wr_sb", [128, 8 * 32], F32)
    modT = sb(gl, "modT", [128, 96], F32)
    A1 = sb(gl, "A1", [128, 16], F32)
    A2 = sb(gl, "A2", [128, 8], F32)
    small = sb(gl, "small", [128, 16], F32)
    neglam = sb(gl, "neglam", [128, 1], F32)
    sm_r, mod_r, bc_r = Res(True), Res(), Res()

    def ld(dst, src, r=None):
        return k.dma(SP, "cst", lambda: nc.sync.dma_start(out=dst, in_=src), writes=[r or sm_r])

    ld(cT[:], cT_d[:, :])
    ld(badaT[:], badaT_d[:, :])
    ld(gT[:], gT_d[:, :])
    ld(gains[:], gains_d[0:1, :].partition_broadcast(128))
    ld(lamv[:], lamv_d[0:1, :].partition_broadcast(128))
    ld(sublnc[:], subln_d[:, :])
    ld(brt[:], br_d[0:1, :].partition_broadcast(128))
    ld(bstart[:], bstart_d[0:1, :].partition_broadcast(128))
    ld(basepk[:], basepk_d[:, :])
    ld(wr_sb[:], wr_d[:, :])

    win_r, wout_r = Res(True), Res(True)

    with ExitStack() as ph:
        wa = [sb(ph, f"wa{i}", [128, 8, 1024], F32) for i in range(2)]
        wa_r = [Res(), Res()]
        ps_mod = ps(ph, "ps_mod", [128, 96], F32)
        junk64 = sb(ph, "junk64", [128, 64], F32)
        psm_r, j_r = Res(), Res()
        k.op(A, lambda: a_.activation(out=scT[:], in_=cT[:], func=AF.Silu), reads=[sm_r], writes=[mod_r])
        for ch in range(6):
            w = wa[ch % 2]
            k.dma(SP, f"wada{ch % 2}", lambda w=w, ch=ch: nc.sync.dma_start(
                out=w[:], in_=w_ada_d[:, ch * 1024:(ch + 1) * 1024].rearrange("(k p) c -> p k c", p=128)),
                writes=[wa_r[ch % 2]])
            for jj in range(8):
                j = ch * 8 + jj
                for kk in range(8):
                    k.op(T, lambda w=w, jj=jj, kk=kk, j=j: t_.matmul(
                        ps_mod[:, j * 2:(j + 1) * 2], lhsT=w[:, kk, jj * 128:(jj + 1) * 128],
                        rhs=scT[:, kk * 2:(kk + 1) * 2], start=(kk == 0), stop=(kk == 7)),
                        reads=[wa_r[ch % 2], mod_r], writes=[psm_r], mark=(kk == 7))
        k.op(V, lambda: v_.tensor_tensor(out=modT[:].rearrange("p (j v) -> p j v", v=2),
                                         in0=ps_mod[:].rearrange("p (j v) -> p j v", v=2),
                                         in1=badaT[:].unsqueeze(2).to_broadcast([128, 48, 2]), op=ALU.add),
             reads=[psm_r, sm_r], writes=[mod_r])
        modv = modT[:].rearrange("p (j v) -> p j v", v=2)
        for vv in range(2):
            k.op(V, lambda vv=vv: v_.scalar_tensor_tensor(out=A1[:, vv * 8:(vv + 1) * 8], in0=modv[:, 8:16, vv], scalar=1.0,
                                                         in1=gT[:, 0:8], op0=ALU.add, op1=ALU.mult),
                 reads=[mod_r, sm_r], writes=[mod_r])
        k.op(V, lambda: v_.scalar_tensor_tensor(out=A2[:], in0=modv[:, 32:40, 0], scalar=1.0, in1=gT[:, 8:16],
                                                op0=ALU.add, op1=ALU.mult), reads=[mod_r, sm_r], writes=[mod_r])
        for i in range(2):
            k.op(V, lambda i=i: v_.tensor_tensor(out=junk64[:], in0=lamv[:, i * 128:i * 128 + 64],
                                                in1=lamv[:, i * 128 + 64:i * 128 + 128], op=ALU.mult),
                 reads=[sm_r], writes=[j_r])
            k.op(V, lambda i=i: v_.tensor_reduce(out=small[:, i:i + 1], in_=junk64[:], axis=AX.X, op=ALU.add),
                 reads=[j_r], writes=[mod_r])
        k.op(A, lambda: a_.activation(out=small[:, 2:4], in_=small[:, 0:2], func=AF.Exp), reads=[mod_r], writes=[mod_r])
        k.op(V, lambda: v_.tensor_tensor(out=small[:, 4:5], in0=small[:, 3:4], in1=small[:, 2:3], op=ALU.subtract),
             reads=[mod_r], writes=[mod_r])
        k.op(V, lambda: v_.tensor_scalar(out=neglam[:], in0=small[:, 4:5], scalar1=-LAM_INIT, scalar2=None, op0=ALU.add),
             reads=[mod_r], writes=[mod_r])
        k.op(V, lambda: v_.tensor_scalar(out=sublnc[:], in0=sublnc[:], scalar1=(1.0 - LAM_INIT), scalar2=None, op0=ALU.mult),
             reads=[sm_r], writes=[sm_r])
        k.barrier()

    def rstd_from_ss(ss_ap, out_ap, n, res):
        k.op(A, lambda: a_.activation(out=out_ap, in_=ss_ap, func=AF.Sqrt, scale=1.0 / n, bias=epsc[:, 0:1]),
             reads=[res, cst_r], writes=[res])
        k.op(V, lambda: v_.reciprocal(out=out_ap, in_=out_ap), reads=[res], writes=[res])


    pc_es = ExitStack()
    pcs = [sb(pc_es, f"pcs{i}", [128, 2048], BF16) for i in range(2)]
    pcs_r = [Res(), Res()]

    with ExitStack() as na:
        KnT = sb(na, "KnT", [128, 4, 38 * 128], BF16)
        Vn = sb(na, "Vn", [128, 38, 8, 65], BF16)
        kn_r, vn_r = Res(), Res()
        k.op(P, lambda: g_.memset(Vn[:], 1.0), writes=[vn_r])

        with ExitStack() as ph:
            w_in_sb = sb(ph, "w_in_sb", [128, 8, 3 * D], BF16)
            for kk in range(8):
                for hh in range(2):
                    k.dma(P, "wld", lambda kk=kk, hh=hh: g_.dma_start(out=w_in_sb[:, kk, hh * 1536:(hh + 1) * 1536],
                                                                    in_=w_in_d[kk * 128:(kk + 1) * 128, hh * 1536:(hh + 1) * 1536]),
                          writes=[win_r])
            pc_chunks = [(wgu_d, wgu_b, ch * 128, 1, 2 * D) for ch in range(256)] + [(wd_d, wd_b, ch * 256, 2, D) for ch in range(128)]
            prev = None
            for n, (src_t, dst_t, r0, rr_, cc_) in enumerate(pc_chunks):
                sl = n % 2
                nrow = 128 * rr_
                k.dma(P, f"pci{sl}", lambda src_t=src_t, r0=r0, rr_=rr_, sl=sl, nrow=nrow: g_.dma_start(
                    out=pcs[sl][:].rearrange("p (r c) -> p r c", r=rr_),
                    in_=src_t[r0:r0 + nrow, :].rearrange("(p r) c -> p r c", r=rr_), max_dma_last_dim=8192),
                    writes=[pcs_r[sl]])
                if prev is not None:
                    prev()
                prev = (lambda dst_t=dst_t, r0=r0, rr_=rr_, sl=sl, nrow=nrow: k.dma(
                    P, f"pco{sl}", lambda: g_.dma_start(out=dst_t[r0:r0 + nrow, :].rearrange("(p r) c -> p r c", r=rr_),
                                                        in_=pcs[sl][:].rearrange("p (r c) -> p r c", r=rr_)),
                    reads=[pcs_r[sl]], writes=[wb_r]))
            prev()
            xin = [sb(ph, f"xin{i}", [128, D], F32) for i in range(3)]
            ropb = [sb(ph, f"ropb{i}", [128, 128], F32) for i in range(3)]
            xin_r, rop_r = [Res() for _ in range(3)], [Res() for _ in range(3)]
            stat = [sb(ph, f"stat{i}", [128, 8], F32) for i in range(2)]
            stat_r = [Res(), Res()]
            xh = [sb(ph, f"xh{i}", [128, D], BF16) for i in range(2)]
            xh_r = [Res(), Res()]
            ps_t = ps(ph, "ps_t", [128, D], BF16)
            pst_r = Res()
            hT = [sb(ph, f"hT{i}", [128, 8, 128], BF16) for i in range(2)]
            hT_r = [Res(), Res()]
            ps_pj = [ps(ph, f"ps_pj{i}", [128, 512], F32) for i in range(3)]
            pspj_r = [Res() for _ in range(3)]
            NW = 2
            kf = [sb(ph, f"kf{i}", [128, 512], F32) for i in range(NW)]
            sq = [sb(ph, f"sq{i}", [128, 512], F32) for i in range(NW)]
            s8 = [sb(ph, f"s8{i}", [128, 16], F32) for i in range(NW)]
            kn3 = [sb(ph, f"kn3{i}", [128, 512], F32) for i in range(NW)]
            t1 = [sb(ph, f"t1{i}", [128, 512], F32) for i in range(NW)]
            kb = [sb(ph, f"kb{i}", [128, 512], BF16) for i in range(NW)]
            kf_r = [Res() for _ in range(NW)]
            sq_r = [Res() for _ in range(NW)]
            s8_r = [Res() for _ in range(NW)]
            kn3_r = [Res() for _ in range(NW)]
            t1_r = [Res() for _ in range(NW)]
            kb_r = [Res() for _ in range(NW)]
            ps_tr = [ps(ph, f"ps_tr{i}", [128, 512], BF16) for i in range(2)]
            pstr_r = [Res(), Res()]
            oT = [sb(ph, f"oT{i}", [128, 4, 128], BF16) for i in range(3)]
            oT_r = [Res() for _ in range(3)]
            cnt = {"pj": 0, "w": 0, "tr": 0, "o": 0}

            def load_x(ti):
                p = ti % 3
                k.dma(SP, f"xld{p}", lambda: nc.sync.dma_start(out=xin[p][:], in_=x_all[ti * 128:(ti + 1) * 128, :]),
                      writes=[xin_r[p]])
                if ti < 64:
                    k.dma(SP, f"rld{p}", lambda: nc.sync.dma_start(out=ropb[p][:], in_=rope[ti * 128:(ti + 1) * 128, :]),
                          writes=[rop_r[p]])

            def front(ti):
                p = ti % 2
                x3 = ti % 3
                vsel = 1 if ti in (64, 65) else 0
                k.op(A, lambda: a_.activation(out=xh[p][:], in_=xin[x3][:], func=AF.Square, accum_out=stat[p][:, 0:1]),
                     reads=[xin_r[x3]], writes=[xh_r[p], stat_r[p]])
                rstd_from_ss(stat[p][:, 0:1], stat[p][:, 1:2], D, stat_r[p])
                k.op(V, lambda: v_.tensor_scalar(out=xh[p][:], in0=xin[x3][:], scalar1=stat[p][:, 1:2], scalar2=None,
                                                 op0=ALU.mult), reads=[xin_r[x3], stat_r[p]], writes=[xh_r[p]])
                for kk in range(8):
                    k.op(T, lambda kk=kk: t_.transpose(out=ps_t[:, kk * 128:(kk + 1) * 128], in_=xh[p][:, kk * 128:(kk + 1) * 128],
                                                       identity=ident_b[:]), reads=[xh_r[p], cst_r], writes=[pst_r],
                         mark=(kk == 7))
                for kk in range(8):
                    k.op(A, lambda kk=kk: a_.activation(out=hT[p][:, kk, :], in_=ps_t[:, kk * 128:(kk + 1) * 128],
                                                        func=AF.Identity, scale=A1[:, vsel * 8 + kk:vsel * 8 + kk + 1],
                                                        bias=modT[:, kk * 2 + vsel:kk * 2 + vsel + 1]),
                         reads=[pst_r, mod_r], writes=[hT_r[p]])

            vb16 = [sb(ph, f"vb16{i}", [128, 512], BF16) for i in range(2)]
            vb16_r = [Res(), Res()]

            def norm_group(items):
                ws = []
                for _ in items:
                    ws.append(cnt["w"] % NW)
                    cnt["w"] += 1
                for (pj, gi, rp), w in zip(items, ws):
                    k.op(A, lambda w=w, pj=pj: a_.copy(out=kf[w][:], in_=ps_pj[pj][:]), reads=[pspj_r[pj]], writes=[kf_r[w]])
                for (pj, gi, rp), w in zip(items, ws):
                    k.op(V, lambda w=w: v_.tensor_tensor(out=sq[w][:], in0=kf[w][:], in1=kf[w][:], op=ALU.mult),
                         reads=[kf_r[w]], writes=[sq_r[w]])
                for (pj, gi, rp), w in zip(items, ws):
                    k.op(V, lambda w=w: v_.tensor_reduce(out=s8[w][:, 0:8], in_=sq[w][:].rearrange("p (g d) -> p g d", d=64),
                                                         axis=AX.X, op=ALU.add), reads=[sq_r[w]], writes=[s8_r[w]])
                for (pj, gi, rp), w in zip(items, ws):
                    k.op(A, lambda w=w: a_.activation(out=s8[w][:, 8:16], in_=s8[w][:, 0:8], func=AF.Sqrt, scale=1.0 / 64,
                                                      bias=epsc[:, 0:1]), reads=[s8_r[w], cst_r], writes=[s8_r[w]])
                for (pj, gi, rp), w in zip(items, ws):
                    k.op(V, lambda w=w: v_.reciprocal(out=s8[w][:, 8:16], in_=s8[w][:, 8:16]), reads=[s8_r[w]], writes=[s8_r[w]])
                for (pj, gi, rp), w in zip(items, ws):
                    k.op(V, lambda w=w: v_.tensor_tensor(out=sq[w][:].rearrange("p (g d) -> p g d", d=64),
                                                         in0=kf[w][:].rearrange("p (g d) -> p g d", d=64),
                                                         in1=s8[w][:, 8:16].unsqueeze(2).to_broadcast([128, 8, 64]), op=ALU.mult),
                         reads=[kf_r[w], s8_r[w]], writes=[sq_r[w]])
                for (pj, gi, rp), w in zip(items, ws):
                    gsl = gains[:, gi * 512:(gi + 1) * 512]
                    if rp is None:
                        k.op(V, lambda w=w, gsl=gsl: v_.tensor_tensor(out=kb[w][:], in0=sq[w][:], in1=gsl, op=ALU.mult),
                             reads=[sq_r[w], sm_r], writes=[kb_r[w]])
                    else:
                        k.op(V, lambda w=w, gsl=gsl: v_.tensor_tensor(out=kn3[w][:], in0=sq[w][:], in1=gsl, op=ALU.mult),
                             reads=[sq_r[w], sm_r], writes=[kn3_r[w]])
                roped = [(rp, w) for (pj, gi, rp), w in zip(items, ws) if rp is not None]
                for rp, w in roped:
                    k.op(V, lambda w=w, rp=rp: v_.tensor_tensor(out=t1[w][:].rearrange("p (g d) -> p g d", d=64),
                                                                in0=kn3[w][:].rearrange("p (g d) -> p g d", d=64),
                                                                in1=ropb[rp][:, 0:64].unsqueeze(1).to_broadcast([128, 8, 64]),
                                                                op=ALU.mult),
                         reads=[kn3_r[w], rop_r[rp]], writes=[t1_r[w]])
                for pr in range(2):
                    for rp, w in roped:
                        kv = kn3[w][:].rearrange("p (g h t i) -> p g h t i", g=8, h=2, t=2, i=16)
                        sv = sq[w][:].rearrange("p (g h t i) -> p g h t i", g=8, h=2, t=2, i=16)
                        sinv = ropb[rp][:, 64:128].rearrange("p (h t i) -> p h t i", h=2, t=2, i=16)
                        k.op(V, lambda pr=pr, kv=kv, sv=sv, sinv=sinv: v_.tensor_tensor(
                            out=sv[:, :, :, pr, :], in0=kv[:, :, :, 1 - pr, :],
                            in1=sinv[:, :, pr, :].unsqueeze(1).to_broadcast([128, 8, 2, 16]), op=ALU.mult),
                            reads=[kn3_r[w], rop_r[rp]], writes=[sq_r[w]])
                for rp, w in roped:
                    k.op(V, lambda w=w: v_.tensor_tensor(out=kb[w][:], in0=t1[w][:], in1=sq[w][:], op=ALU.add),
                         reads=[t1_r[w], sq_r[w]], writes=[kb_r[w]])
                return [(kb[w], kb_r[w]) for w in ws]

            def transpose4(src, src_r):
                r = cnt["tr"] % 2
                cnt["tr"] += 1
                for blk in range(4):
                    k.op(T, lambda blk=blk: t_.transpose(out=ps_tr[r][:, blk * 128:(blk + 1) * 128],
                                                         in_=src[:, blk * 128:(blk + 1) * 128], identity=ident_b[:]),
                         reads=[src_r, cst_r], writes=[pstr_r[r]], mark=(blk == 3))
                return r

            def proj(p, c):
                pj = cnt["pj"] % 3
                cnt["pj"] += 1
                for kk in range(8):
                    k.op(T, lambda kk=kk: t_.matmul(ps_pj[pj][:], lhsT=hT[p][:, kk, :], rhs=w_in_sb[:, kk, c * 512:(c + 1) * 512],
                                                    start=(kk == 0), stop=(kk == 7)),
                         reads=[hT_r[p], win_r], writes=[pspj_r[pj]], mark=(kk == 7))
                return pj

            def post_scratch(ti, src, src_r, dst, dres):
                r = transpose4(src, src_r)
                o = cnt["o"] % 3
                cnt["o"] += 1
                k.op(A, lambda: a_.copy(out=oT[o][:].rearrange("p h t -> p (h t)"), in_=ps_tr[r][:]),
                     reads=[pstr_r[r]], writes=[oT_r[o]])
                k.dma(SP, f"kst{o}", lambda: nc.sync.dma_start(
                    out=dst.rearrange("h p c -> p h c")[:, :, ti * 128:(ti + 1) * 128], in_=oT[o][:]),
                    reads=[oT_r[o]], writes=[dres])

            def post_kn(lna, src, src_r):
                r = transpose4(src, src_r)
                k.op(A, lambda: a_.copy(out=KnT[:, :, lna * 128:(lna + 1) * 128],
                                        in_=ps_tr[r][:].rearrange("p (h t) -> p h t", h=4)),
                     reads=[pstr_r[r]], writes=[kn_r])

            def evac_vd(ti, pj):
                w = cnt["v"] % 2
                cnt["v"] += 1
                k.op(A, lambda: a_.copy(out=vb16[w][:], in_=ps_pj[pj][:]), reads=[pspj_r[pj]], writes=[vb16_r[w]])
                k.dma(SP, f"vst{w}", lambda: nc.sync.dma_start(
                    out=vd_s.rearrange("h p (t e) -> p h t e", e=128)[:, :, ti, :],
                    in_=vb16[w][:].rearrange("p (h e) -> p h e", h=4)), reads=[vb16_r[w]], writes=[vd_r])

            def evac_vn(lna, pj):
                k.op(A, lambda: a_.copy(out=Vn[:, lna, :, 0:64], in_=ps_pj[pj][:].rearrange("p (h e) -> p h e", h=8)),
                     reads=[pspj_r[pj]], writes=[vn_r])

            cnt["v"] = 0
            load_x(0)
            load_x(1)
            front(0)
            for ti in range(NT_ALL):
                p = ti % 2
                x3 = ti % 3
                own = ti < 32
                ctx = ti in (64, 65)
                halo = ti >= 66
                if ti + 2 < NT_ALL:
                    load_x(ti + 2)
                nxt = (lambda: front(ti + 1)) if ti + 1 < NT_ALL else (lambda: None)
                if own:
                    lna = ti + 2
                    pq_, pk_ = proj(p, 0), proj(p, 1)
                    nxt()
                    (qd_t, qd_r2), (kd_t, kd_r2) = norm_group([(pq_, 0, x3), (pk_, 1, x3)])
                    evac_vd(ti, proj(p, 2))
                    pq_, pk_ = proj(p, 3), proj(p, 4)
                    post_scratch(ti, qd_t, qd_r2, qdt_s, qdt_r)
                    post_scratch(ti, kd_t, kd_r2, kdt_s, kdt_r)
                    (qn_t2, qn_r2), (kn_t2, kn_r2) = norm_group([(pq_, 2, None), (pk_, 3, None)])
                    evac_vn(lna, proj(p, 5))
                    post_scratch(ti, qn_t2, qn_r2, qnt_s, qnt_r)
                    post_kn(lna, kn_t2, kn_r2)
                elif ctx:
                    lna = 36 + (ti - 64)
                    pk_, pn_ = proj(p, 1), proj(p, 4)
                    nxt()
                    (kd_t, kd_r2), (kn_t2, kn_r2) = norm_group([(pk_, 1, None), (pn_, 3, None)])
                    post_scratch(ti, kd_t, kd_r2, kdt_s, kdt_r)
                    post_kn(lna, kn_t2, kn_r2)
                    evac_vd(ti, proj(p, 2))
                    evac_vn(lna, proj(p, 5))
                elif halo:
                    lna = [0, 1, 34, 35][ti - 66]
                    pn_, pv_ = proj(p, 4), proj(p, 5)
                    nxt()
                    ((kn_t2, kn_r2),) = norm_group([(pn_, 3, None)])
                    post_kn(lna, kn_t2, kn_r2)
                    evac_vn(lna, pv_)
                else:
                    pk_, pv_ = proj(p, 1), proj(p, 2)
                    nxt()
                    ((kd_t, kd_r2),) = norm_group([(pk_, 1, x3)])
                    post_scratch(ti, kd_t, kd_r2, kdt_s, kdt_r)
                    evac_vd(ti, pv_)
            k.barrier()

        with ExitStack() as ph:
            biasm = sb(ph, "biasm", [128, 6, 8, 128], F32)
            bias_r = Res()
            ps_s = [[ps(ph, f"ps_s{i}{j}", [128, 4, 128], F32) for j in range(2)] for i in range(2)]
            pss_r = [Res(), Res()]
            ps_o = [ps(ph, f"ps_o{i}", [128, 4, 128], F32) for i in range(2)]
            pso_r = [Res(), Res()]
            tmpS = [sb(ph, f"tmpS{i}", [128, 6, 128], F32) for i in range(2)]
            tmpS_r = [Res(), Res()]
            PT = [sb(ph, f"PTn{i}", [128, 8, 128], BF16) for i in range(2)]
            PT_r = [Res(), Res()]
            rec = sb(ph, "rec", [128, 16], F32)
            of = sb(ph, "of", [128, 8, 64], F32)
            osq = sb(ph, "osq", [128, 8, 64], F32)
            ep_r = Res()
            qn_t = [sb(ph, f"qn_t{i}", [128, 4, 128], BF16) for i in range(2)]
            qnt_tr = [Res(), Res()]
            mxs = [sb(ph, f"mxs{i}", [128, 512], BF16) for i in range(2)]
            mxs_r = [Res(), Res()]

            def ldq(i):
                k.dma(SP, f"qnld{i % 2}", lambda: nc.sync.dma_start(out=qn_t[i % 2][:],
                                                           in_=qnt_s.rearrange("h p c -> p h c")[:, :, i * 128:(i + 1) * 128]),
                      reads=[qnt_r], writes=[qnt_tr[i % 2]])
            ldq(0)
            slot_of = lambda i: 0 if i == 0 else 1 if i == 1 else 3 if i == 30 else 4 if i == 31 else 2
            state = {"slot": -1}

            def tiles_of(i):
                if i == 0:
                    win = list(range(0, 6))
                elif i == 31:
                    win = list(range(30, 36))
                else:
                    win = list(range(i, i + 5))
                return len(win), win + [36, 37]

            def front(u):
                i, h = divmod(u, 8)
                b2 = u % 2
                if h == 0:
                    if i + 1 < NT_OWN:
                        ldq(i + 1)
                    sl = slot_of(i)
                    if sl != state["slot"]:
                        state["slot"] = sl
                        k.dma(SP, "bld", lambda: nc.sync.dma_start(out=biasm[:].rearrange("p w h q -> p (w h q)"),
                                                                   in_=biasm_d[sl, :, :]), writes=[bias_r])
                nW, tiles = tiles_of(i)
                pair, part = h // 2, (h % 2) * 64
                for w, l in enumerate(tiles):
                    k.op(T, lambda w=w, l=l: t_.matmul(ps_s[b2][w // 4][:, w % 4, :],
                                                       lhsT=KnT[part:part + 64, pair, l * 128:(l + 1) * 128],
                                                       rhs=qn_t[i % 2][part:part + 64, pair, :],
                                                       start=True, stop=True),
                         reads=[kn_r, qnt_tr[i % 2]], writes=[pss_r[b2]], mark=(w == len(tiles) - 1))
                k.op(V, lambda: v_.scalar_tensor_tensor(out=tmpS[b2][:, 0:4, :], in0=ps_s[b2][0][:, 0:4, :], scalar=0.125,
                                                        in1=biasm[:, 0:4, h, :], op0=ALU.mult, op1=ALU.add),
                     reads=[pss_r[b2], bias_r], writes=[tmpS_r[b2]])
                k.op(V, lambda: v_.scalar_tensor_tensor(out=tmpS[b2][:, 4:nW, :], in0=ps_s[b2][1][:, 0:nW - 4, :], scalar=0.125,
                                                        in1=biasm[:, 4:nW, h, :], op0=ALU.mult, op1=ALU.add),
                     reads=[pss_r[b2], bias_r], writes=[tmpS_r[b2]])
                k.op(A, lambda: a_.activation(out=PT[b2][:, 0:nW, :], in_=tmpS[b2][:, 0:nW, :], func=AF.Exp),
                     reads=[tmpS_r[b2]], writes=[PT_r[b2]])
                k.op(A, lambda: a_.activation(out=PT[b2][:, nW:nW + 2, :], in_=ps_s[b2][1][:, nW - 4:nW - 2, :],
                                              func=AF.Exp, scale=0.125), reads=[pss_r[b2]], writes=[PT_r[b2]])

            def back(u):
                i, h = divmod(u, 8)
                b2 = u % 2
                nW, tiles = tiles_of(i)
                for w, l in enumerate(tiles):
                    k.op(T, lambda w=w, l=l: t_.matmul(ps_o[h // 4][:, h % 4, 0:65], lhsT=PT[b2][:, w, :],
                                                       rhs=Vn[:, l, h, :], start=(w == 0), stop=(w == len(tiles) - 1)),
                         reads=[PT_r[b2], vn_r], writes=[pso_r[h // 4]], mark=(w == len(tiles) - 1))
                if h != 7:
                    return
                for hb in range(2):
                    k.op(V, lambda hb=hb: v_.reciprocal(out=rec[:, hb * 4:(hb + 1) * 4], in_=ps_o[hb][:, :, 64]),
                         reads=[pso_r[hb]], writes=[ep_r])
                    k.op(V, lambda hb=hb: v_.tensor_tensor(out=of[:, hb * 4:(hb + 1) * 4, :], in0=ps_o[hb][:, :, 0:64],
                                                           in1=rec[:, hb * 4:(hb + 1) * 4].unsqueeze(2).to_broadcast([128, 4, 64]),
                                                           op=ALU.mult), reads=[pso_r[hb], ep_r], writes=[ep_r])
                k.op(V, lambda: v_.tensor_tensor(out=osq[:], in0=of[:], in1=of[:], op=ALU.mult), reads=[ep_r], writes=[ep_r])
                k.op(V, lambda: v_.tensor_reduce(out=rec[:, 0:8], in_=osq[:], axis=AX.X, op=ALU.add), reads=[ep_r], writes=[ep_r])
                rstd_from_ss(rec[:, 0:8], rec[:, 8:16], 64, ep_r)
                k.op(V, lambda: v_.tensor_tensor(out=osq[:], in0=of[:], in1=rec[:, 8:16].unsqueeze(2).to_broadcast([128, 8, 64]),
                                                 op=ALU.mult), reads=[ep_r], writes=[ep_r])
                k.op(V, lambda: v_.tensor_tensor(out=mxs[i % 2][:], in0=osq[:].rearrange("p h d -> p (h d)"),
                                                 in1=gains[:, 4 * 512:5 * 512], op=ALU.mult),
                     reads=[ep_r, sm_r], writes=[mxs_r[i % 2]])
                k.dma(SP, f"mxst{i % 2}", lambda: nc.sync.dma_start(out=mixn_s[i * 128:(i + 1) * 128, :], in_=mxs[i % 2][:]),
                      reads=[mxs_r[i % 2]], writes=[mixn_r])

            NU = NT_OWN * 8
            front(0)
            for u in range(NU):
                if u + 1 < NU:
                    front(u + 1)
                back(u)
            k.barrier()

    with ExitStack() as ph:
        KT = [sb(ph, f"KT{i}", [128, NKEY], BF16) for i in range(2)]
        VV = [sb(ph, f"VV{i}", [128, NT_KEY, 128], BF16) for i in range(2)]
        QT = [sb(ph, f"QT{i}", [128, NQ], BF16) for i in range(2)]
        kvq_r = [Res(), Res()]
        NS = 6
        ps_s = [ps(ph, f"pd_s{i}", [128, 512], F32) for i in range(NS)]
        pss_r = [Res() for _ in range(NS)]
        ps_ot = [ps(ph, f"pd_o{i}", [128, 512], F32) for i in range(2)]
        pso_r = [Res(), Res()]
        NP = 8
        PTd = [sb(ph, f"PTd{i}", [128, 512], BF16) for i in range(NP)]
        PTd_r = [Res() for _ in range(NP)]
        accD = [sb(ph, f"accD{i}", [128, 512], F32) for i in range(2)]
        accD_r = [Res(), Res()]
        tpa = [[sb(ph, f"tpa{i}{j}", [128, 512], BF16) for j in range(3)] for i in range(2)]
        tpa_r = [[Res() for j in range(3)] for i in range(2)]
        rd = [sb(ph, f"rd{i}", [128, 512], F32) for i in range(2)]
        om = [sb(ph, f"om{i}", [128, 512], F32) for i in range(2)]
        dd = sb(ph, "dd", [128, 512], F32)
        dsq = sb(ph, "dsq", [128, 512], F32)
        rr = sb(ph, "rr", [128, 512], F32)
        e_r = Res()
        mds = [sb(ph, f"mds{i}", [128, 512], BF16) for i in range(2)]
        mds_r = [Res(), Res()]
        nmd = [0]

        def load_head(h):
            b2 = h % 2
            for q4 in range(4):
                c0, c1 = q4 * (NKEY // 4), (q4 + 1) * (NKEY // 4)
                k.dma(SP, f"hld{b2}", lambda c0=c0, c1=c1: nc.sync.dma_start(out=KT[b2][:, c0:c1], in_=kdt_s[h, :, c0:c1]),
                      reads=[kdt_r], writes=[kvq_r[b2]])
                k.dma(SP, f"hld{b2}", lambda c0=c0, c1=c1: nc.sync.dma_start(
                    out=VV[b2][:].rearrange("p t e -> p (t e)")[:, c0:c1], in_=vd_s[h, :, c0:c1]),
                    reads=[vd_r], writes=[kvq_r[b2]])
            k.dma(SP, f"hld{b2}", lambda: nc.sync.dma_start(out=QT[b2][:], in_=qdt_s[h, :, :]), reads=[qdt_r], writes=[kvq_r[b2]])

        load_head(0)
        sc = 0
        for h in range(4):
            b2 = h % 2
            if h + 1 < 4:
                load_head(h + 1)
            for c in range(8):
                base = sc

                def S(kt, m):
                    s_ = (base + 2 * kt + m) % NS
                    mp = m * 64
                    k.op(T, lambda: t_.matmul(ps_s[s_][:], lhsT=KT[b2][mp:mp + 64, kt * 128:(kt + 1) * 128],
                                              rhs=QT[b2][mp:mp + 64, c * 512:(c + 1) * 512], start=True, stop=True),
                         reads=[kvq_r[b2]], writes=[pss_r[s_]])

                def EX(kt, m):
                    s_ = (base + 2 * kt + m) % NS
                    pq = (base + 2 * kt + m) % NP
                    k.op(A, lambda: a_.activation(out=PTd[pq][:], in_=ps_s[s_][:], func=AF.Exp, scale=0.125),
                         reads=[pss_r[s_]], writes=[PTd_r[pq]])

                def PV(kt, m):
                    u = 2 * kt + m
                    pq = (base + u) % NP
                    k.op(T, lambda: t_.matmul(ps_ot[m][:], lhsT=VV[b2][:, kt, :], rhs=PTd[pq][:],
                                              start=(kt == 0), stop=(kt == NT_KEY - 1)),
                         reads=[kvq_r[b2], PTd_r[pq]], writes=[pso_r[m]])
                    if kt % 2 == 1:
                        pp = (base + u - 2) % NP
                        j = (kt % 4) // 2
                        k.op(V, lambda: v_.tensor_tensor(out=tpa[m][j][:], in0=PTd[pp][:], in1=PTd[pq][:], op=ALU.add),
                             reads=[PTd_r[pp], PTd_r[pq]], writes=[tpa_r[m][j]])
                        src, src_r = None, None
                        if kt % 4 == 3:
                            k.op(V, lambda: v_.tensor_tensor(out=tpa[m][2][:], in0=tpa[m][0][:], in1=tpa[m][1][:], op=ALU.add),
                                 reads=[tpa_r[m][0], tpa_r[m][1]], writes=[tpa_r[m][2]])
                            src, src_r = tpa[m][2], tpa_r[m][2]
                        elif kt == NT_KEY - 1:
                            src, src_r = tpa[m][j], tpa_r[m][j]
                        if src is not None:
                            if kt == 3:
                                k.op(V, lambda: v_.tensor_copy(out=accD[m][:], in_=src[:]), reads=[src_r], writes=[accD_r[m]])
                            else:
                                k.op(V, lambda: v_.tensor_tensor(out=accD[m][:], in0=accD[m][:], in1=src[:], op=ALU.add),
                                     reads=[src_r, accD_r[m]], writes=[accD_r[m]])

                LA = 2
                for kt in range(LA):
                    for m in range(2):
                        S(kt, m)
                    for m in range(2):
                        EX(kt, m)
                for kt in range(NT_KEY):
                    if kt + LA < NT_KEY:
                        for m in range(2):
                            S(kt + LA, m)
                        for m in range(2):
                            EX(kt + LA, m)
                    for m in range(2):
                        PV(kt, m)
                sc += 2 * NT_KEY
                for m in range(2):
                    s_ = sc % NS
                    sc += 1
                    k.op(T, lambda m=m, s_=s_: t_.matmul(ps_s[s_][:], lhsT=ones_f[:], rhs=accD[m][:], start=True, stop=True),
                         reads=[accD_r[m], cst_r], writes=[pss_r[s_]])
                    k.op(V, lambda m=m, s_=s_: v_.reciprocal(out=rd[m][:], in_=ps_s[s_][:]), reads=[pss_r[s_]], writes=[e_r])
                    k.op(V, lambda m=m: v_.tensor_tensor(out=om[m][:], in0=ps_ot[m][:], in1=rd[m][:], op=ALU.mult),
                         reads=[pso_r[m], e_r], writes=[e_r])
                k.op(V, lambda: v_.scalar_tensor_tensor(out=dd[:], in0=om[1][:], scalar=neglam[:, 0:1], in1=om[0][:],
                                                        op0=ALU.mult, op1=ALU.add), reads=[e_r, mod_r], writes=[e_r])
                k.op(V, lambda: v_.tensor_tensor(out=dsq[:], in0=dd[:], in1=dd[:], op=ALU.mult), reads=[e_r], writes=[e_r])
                s_ = sc % NS
                sc += 1
                k.op(T, lambda: t_.matmul(ps_s[s_][:], lhsT=ones_f[:], rhs=dsq[:], start=True, stop=True),
                     reads=[e_r, cst_r], writes=[pss_r[s_]])
                k.op(A, lambda: a_.activation(out=rr[:], in_=ps_s[s_][:], func=AF.Sqrt, scale=1.0 / 128, bias=epsc[:, 0:1]),
                     reads=[pss_r[s_], cst_r], writes=[e_r])
                k.op(V, lambda: v_.reciprocal(out=rr[:], in_=rr[:]), reads=[e_r], writes=[e_r])
                mi = nmd[0] % 2
                nmd[0] += 1
                k.op(V, lambda: v_.scalar_tensor_tensor(out=mds[mi][:], in0=dd[:], scalar=sublnc[:, 0:1],
                                                        in1=rr[:], op0=ALU.mult, op1=ALU.mult),
                     reads=[e_r, sm_r], writes=[mds_r[mi]])
                k.dma(SP, f"mdst{mi}", lambda: nc.sync.dma_start(out=mixT_s[h, :, c * 512:(c + 1) * 512], in_=mds[mi][:]),
                      reads=[mds_r[mi]], writes=[mixd_r])
        k.barrier()

    k.barrier(full=True)
    pc_es.close()

    moe_es = ExitStack()
    bcs = sb(moe_es, "bcs", [128, 4, D], F32)
    idx_i = sb(moe_es, "idx_i", [128, NT_OWN, 4], I32)
    gk = sb(moe_es, "gk", [128, NT_OWN, 4], F32)
    widx = sb(moe_es, "widx", [128, NBLK, 8], I32)
    ebi = sb(moe_es, "ebi", [128, NBLK], I32)
    bidx = sb(moe_es, "bidx", [128, NBLK], I32)
    ix_r = Res()
    rt_es = ExitStack()
    lg_all = sb(rt_es, "lg_all", [128, NT_OWN, 32], F32)
    M_all = sb(rt_es, "M_all", [128, NT_OWN, 32], F32)
    Gd = sb(rt_es, "Gd", [128, NT_OWN, 32], F32)
    rt_r = Res()
    modv = modT[:].rearrange("p (j v) -> p j v", v=2)
    with ExitStack() as ph:
        ps_bc = [ps(ph, f"ps_bc{i}", [128, 512], F32) for i in range(2)]
        dg = sb(ph, "dg", [128, 8, 128], F32)
        psb_r, dg_r = [Res(), Res()], Res()
        srcs = [lambda kt: modv[:, 16 + kt, 0:1], lambda kt: modv[:, 40 + kt, 0:1],
                lambda kt: A2[:, kt:kt + 1], lambda kt: modv[:, 24 + kt, 0:1]]
        for n in range(4):
            for kt in range(8):
                k.op(V, lambda n=n, kt=kt: v_.tensor_scalar(out=dg[:, kt, :], in0=ident_f[:], scalar1=srcs[n](kt), scalar2=None,
                                                           op0=ALU.mult), reads=[mod_r, cst_r], writes=[dg_r])
            for kt in range(8):
                k.op(T, lambda kt=kt: t_.matmul(ps_bc[kt // 4][:, (kt % 4) * 128:(kt % 4 + 1) * 128], lhsT=ones_f[:],
                                                rhs=dg[:, kt, :], start=True, stop=True),
                     reads=[dg_r, cst_r], writes=[psb_r[kt // 4]])
            for hf in range(2):
                k.op(A, lambda n=n, hf=hf: a_.copy(out=bcs[:, n, hf * 512:(hf + 1) * 512], in_=ps_bc[hf][:]),
                     reads=[psb_r[hf]], writes=[bc_r])
        k.barrier()
    with ExitStack() as ph:
        w_out_sb = sb(ph, "w_out_sb", [128, 8, D], BF16)
        for kk in range(8):
            k.dma(P, "wld", lambda kk=kk: g_.dma_start(out=w_out_sb[:, kk, :], in_=w_out_d[kk * 128:(kk + 1) * 128, :]),
                  writes=[wout_r])
        mxl = [sb(ph, f"mxl{i}", [128, 512], BF16) for i in range(2)]
        mdl = [sb(ph, f"mdl{i}", [128, 4, 128], BF16) for i in range(2)]
        mxl_r, mdl_r = [Res(), Res()], [Res(), Res()]
        h2bt = [sb(ph, f"h2bt{i}", [128, D], BF16) for i in range(2)]
        h2bt_r = [Res(), Res()]
        ps_y = [ps(ph, f"ps_y{i}", [128, 512], F32) for i in range(2)]
        ps_tn = ps(ph, "ps_tn", [128, 512], BF16)
        ps_h2 = [ps(ph, f"ps_h2{i}", [128, 512], F32) for i in range(2)]
        ps_lg = ps(ph, "ps_lg", [128, 32], F32)
        psy_r, pstn_r, psh_r, pslg_r = [Res(), Res()], Res(), [Res(), Res()], Res()
        mixnT = sb(ph, "mixnT", [128, 4, 128], BF16)
        mnT_r = Res()
        xin2 = [sb(ph, f"xin2{i}", [128, D], F32) for i in range(2)]
        xin2_r = [Res(), Res()]
        x1 = [sb(ph, f"x1{i}", [128, D], F32) for i in range(2)]
        x1_r = [Res(), Res()]
        h2f = sb(ph, "h2f", [128, D], F32)
        h2f_r = Res()
        junkb2 = sb(ph, "junkb2", [128, D], BF16)
        jb_r = Res()
        st2 = [sb(ph, f"st2{i}", [128, 16], F32) for i in range(2)]
        st2_r = [Res(), Res()]
        h2T = sb(ph, "h2T", [128, 8, 128], F32)
        h2T_r = Res()
        pex = sb(ph, "pex", [128, 32], F32)
        pm = sb(ph, "pm", [128, 32], F32)
        px_r = Res()

        def ldx2(i):
            k.dma(SP, f"x2a{i % 2}", lambda: nc.sync.dma_start(out=xin2[i % 2][:], in_=x_all[i * 128:(i + 1) * 128, :]),
                  writes=[xin2_r[i % 2]])
            k.dma(SP, f"x2b{i % 2}", lambda: nc.sync.dma_start(out=mxl[i % 2][:], in_=mixn_s[i * 128:(i + 1) * 128, :]),
                  reads=[mixn_r], writes=[mxl_r[i % 2]])
            k.dma(SP, f"x2c{i % 2}", lambda: nc.sync.dma_start(out=mdl[i % 2][:],
                                                       in_=mixT_s.rearrange("h p c -> p h c")[:, :, i * 128:(i + 1) * 128]),
                  reads=[mixd_r], writes=[mdl_r[i % 2]])
        ldx2(0)
        for i in range(NT_OWN):
            p = i % 2
            if i + 1 < NT_OWN:
                ldx2(i + 1)
            for blk in range(4):
                k.op(T, lambda blk=blk: t_.transpose(out=ps_tn[:, blk * 128:(blk + 1) * 128],
                                                     in_=mxl[p][:, blk * 128:(blk + 1) * 128], identity=ident_b[:]),
                     reads=[mxl_r[p], cst_r], writes=[pstn_r], mark=(blk == 3))
            k.op(A, lambda: a_.copy(out=mixnT[:].rearrange("p h t -> p (h t)"), in_=ps_tn[:]), reads=[pstn_r], writes=[mnT_r])
            for hf in range(2):
                for blk in range(8):
                    lh = mdl[p][:, blk, :] if blk < 4 else mixnT[:, blk - 4, :]
                    k.op(T, lambda lh=lh, blk=blk, hf=hf: t_.matmul(ps_y[hf][:], lhsT=lh,
                                                                   rhs=w_out_sb[:, blk, hf * 512:(hf + 1) * 512],
                                                                   start=(blk == 0), stop=(blk == 7)),
                         reads=[mdl_r[p], mnT_r, wout_r], writes=[psy_r[hf]], mark=(blk == 7))
            for hf in range(2):
                sl = slice(hf * 512, (hf + 1) * 512)
                k.op(V, lambda hf=hf, sl=sl: v_.tensor_tensor(out=x1[p][:, sl], in0=ps_y[hf][:], in1=bcs[:, 0, sl], op=ALU.mult),
                     reads=[psy_r[hf], bc_r], writes=[x1_r[p]])
            k.op(V, lambda: v_.tensor_tensor(out=x1[p][:], in0=x1[p][:], in1=xin2[p][:], op=ALU.add),
                 reads=[x1_r[p], xin2_r[p]], writes=[x1_r[p]])
            k.dma(SP, f"x1st{p}", lambda: nc.sync.dma_start(out=out_d[i * 128:(i + 1) * 128, :], in_=x1[p][:]),
                  reads=[x1_r[p]], writes=[out_r[i]])
            k.op(A, lambda: a_.activation(out=junkb2[:], in_=x1[p][:], func=AF.Square, accum_out=st2[p][:, 0:1]),
                 reads=[x1_r[p]], writes=[jb_r, st2_r[p]])
            rstd_from_ss(st2[p][:, 0:1], st2[p][:, 1:2], D, st2_r[p])
            k.op(V, lambda: v_.scalar_tensor_tensor(out=h2f[:], in0=x1[p][:], scalar=st2[p][:, 1:2], in1=bcs[:, 2, :],
                                                    op0=ALU.mult, op1=ALU.mult), reads=[x1_r[p], st2_r[p], bc_r], writes=[h2f_r])
            k.op(V, lambda: v_.tensor_tensor(out=h2f[:], in0=h2f[:], in1=bcs[:, 3, :], op=ALU.add),
                 reads=[h2f_r, bc_r], writes=[h2f_r])
            k.op(A, lambda: a_.copy(out=h2bt[p][:], in_=h2f[:]), reads=[h2f_r], writes=[h2bt_r[p]])
            k.dma(SP, f"h2st{p}", lambda: nc.sync.dma_start(out=h2_s[i * 128:(i + 1) * 128, :], in_=h2bt[p][:]),
                  reads=[h2bt_r[p]], writes=[h2s_r])
            for kk in range(8):
                k.op(T, lambda kk=kk: t_.transpose(out=ps_h2[kk // 4][:, (kk % 4) * 128:(kk % 4 + 1) * 128],
                                                   in_=h2f[:, kk * 128:(kk + 1) * 128], identity=ident_f[:]),
                     reads=[h2f_r, cst_r], writes=[psh_r[kk // 4]], mark=(kk % 4 == 3))
            for hf in range(2):
                k.op(A, lambda hf=hf: a_.copy(out=h2T[:, hf * 4:(hf + 1) * 4, :].rearrange("p k t -> p (k t)"), in_=ps_h2[hf][:]),
                     reads=[psh_r[hf]], writes=[h2T_r])
            for kk in range(8):
                k.op(T, lambda kk=kk: t_.matmul(ps_lg[:], lhsT=h2T[:, kk, :], rhs=wr_sb[:, kk * 32:(kk + 1) * 32],
                                                start=(kk == 0), stop=(kk == 7)),
                     reads=[h2T_r, sm_r], writes=[pslg_r], mark=(kk == 7))
            k.op(V, lambda: v_.tensor_tensor(out=lg_all[:, i, :], in0=ps_lg[:], in1=brt[:], op=ALU.add),
                 reads=[pslg_r, sm_r], writes=[rt_r])
            k.op(V, lambda: v_.max(out=st2[p][:, 8:16], in_=lg_all[:, i, :]), reads=[rt_r], writes=[st2_r[p]])
            k.op(V, lambda: v_.tensor_scalar(out=M_all[:, i, :], in0=lg_all[:, i, :], scalar1=st2[p][:, 11:12], scalar2=None,
                                             op0=ALU.is_ge), reads=[rt_r, st2_r[p]], writes=[rt_r])
            k.op(V, lambda: v_.tensor_scalar(out=st2[p][:, 2:3], in0=st2[p][:, 8:9], scalar1=-1.0, scalar2=None, op0=ALU.mult),
                 reads=[st2_r[p]], writes=[st2_r[p]])
            k.op(A, lambda: a_.activation(out=pex[:], in_=lg_all[:, i, :], func=AF.Exp, bias=st2[p][:, 2:3]),
                 reads=[rt_r, st2_r[p]], writes=[px_r])
            k.op(V, lambda: v_.tensor_tensor(out=pm[:], in0=pex[:], in1=M_all[:, i, :], op=ALU.mult),
                 reads=[px_r, rt_r], writes=[px_r])
            k.op(V, lambda: v_.tensor_reduce(out=st2[p][:, 3:4], in_=pm[:], axis=AX.X, op=ALU.add),
                 reads=[px_r], writes=[st2_r[p]])
            k.op(V, lambda: v_.reciprocal(out=st2[p][:, 4:5], in_=st2[p][:, 3:4]), reads=[st2_r[p]], writes=[st2_r[p]])
            k.op(V, lambda: v_.tensor_scalar(out=Gd[:, i, :], in0=pm[:], scalar1=st2[p][:, 4:5], scalar2=None, op0=ALU.mult),
                 reads=[px_r, st2_r[p]], writes=[rt_r])
        k.barrier()

    with ExitStack() as ph:
        stg = [sb(ph, f"stg{i}", [128, D], BF16) for i in range(2)]
        stg_r = [Res(), Res()]
        M_bf = sb(ph, "M_bf", [128, NT_OWN, 32], BF16)
        ps_rk = [ps(ph, f"ps_rk{i}", [128, 512], F32) for i in range(2)]
        ps_tot = ps(ph, "ps_tot", [128, 32], F32)
        rk_r, tot_r, mb_r = [Res(), Res()], Res(), Res()
        cw = sb(ph, "cw", [128, 8, 32], F32)
        cwi = sb(ph, "cwi", [128, 2, 32], I32)
        c_r = Res()
        pos = sb(ph, "pos", [128, NT_OWN, 32], F32)
        val = sb(ph, "val", [128, NT_OWN, 32], F32)
        v8 = sb(ph, "v8", [128, NT_OWN, 8], F32)
        idxf = sb(ph, "idxf", [128, NT_OWN, 4], F32)
        oh = sb(ph, "oh", [128, 32], F32)
        ohj = sb(ph, "ohj", [128, 32], F32)
        cmp = sb(ph, "cmp", [128, NBLK, 32], F32)
        ebf = sb(ph, "ebf", [128, NBLK], F32)
        wf = sb(ph, "wf", [128, NBLK, 8], F32)
        k.op(V, lambda: v_.tensor_copy(out=M_bf[:], in_=M_all[:]), reads=[rt_r], writes=[mb_r])
        for i in range(NT_OWN):
            bank, col = i // 16, (i % 16) * 32
            for j in range(i):
                k.op(T, lambda j=j, bank=bank, col=col: t_.matmul(ps_rk[bank][:, col:col + 32], lhsT=ones_b[:, 0:128],
                                                                 rhs=M_bf[:, j, :], start=(j == 0), stop=False),
                     reads=[mb_r, cst_r], writes=[rk_r[bank]], mark=False)
            k.op(T, lambda i=i, bank=bank, col=col: t_.matmul(ps_rk[bank][:, col:col + 32], lhsT=U_b[:], rhs=M_bf[:, i, :],
                                                             start=(i == 0), stop=True),
                 reads=[mb_r, cst_r], writes=[rk_r[bank]], mark=True)
        for j in range(NT_OWN):
            k.op(T, lambda j=j: t_.matmul(ps_tot[:], lhsT=ones_b[:, 0:128], rhs=M_bf[:, j, :], start=(j == 0),
                                          stop=(j == NT_OWN - 1)), reads=[mb_r, cst_r], writes=[tot_r], mark=(j == NT_OWN - 1))
        k.op(V, lambda: v_.tensor_scalar(out=cw[:, 0, :], in0=ps_tot[:], scalar1=float(BS - 1), scalar2=None, op0=ALU.add),
             reads=[tot_r], writes=[c_r])
        k.op(V, lambda: v_.tensor_copy(out=cwi[:, 0, :], in_=cw[:, 0, :]), reads=[c_r], writes=[c_r])
        k.op(V, lambda: v_.tensor_scalar(out=cwi[:, 1, :], in0=cwi[:, 0, :], scalar1=9, scalar2=9,
                                         op0=ALU.logical_shift_right, op1=ALU.logical_shift_left), reads=[c_r], writes=[c_r])
        k.op(V, lambda: v_.tensor_copy(out=cw[:, 1, :], in_=cwi[:, 1, :]), reads=[c_r], writes=[c_r])
        k.op(V, lambda: v_.tensor_copy(out=cw[:, 2, :], in_=cw[:, 1, :]), reads=[c_r], writes=[c_r])
        a, b = 2, 3
        for s in (1, 2, 4, 8, 16):
            k.op(V, lambda a=a, b=b, s=s: v_.tensor_copy(out=cw[:, b, 0:s], in_=cw[:, a, 0:s]), reads=[c_r], writes=[c_r])
            k.op(V, lambda a=a, b=b, s=s: v_.tensor_tensor(out=cw[:, b, s:32], in0=cw[:, a, s:32], in1=cw[:, a, 0:32 - s],
                                                           op=ALU.add), reads=[c_r], writes=[c_r])
            a, b = b, a
        pe_i = a
        k.op(V, lambda: v_.tensor_tensor(out=cw[:, 4, :], in0=cw[:, pe_i, :], in1=cw[:, 1, :], op=ALU.subtract),
             reads=[c_r], writes=[c_r])
        for bank in range(2):
            k.op(V, lambda bank=bank: v_.tensor_tensor(
                out=pos[:, bank * 16:(bank + 1) * 16, :], in0=ps_rk[bank][:].rearrange("p (i e) -> p i e", e=32),
                in1=cw[:, 4, :].unsqueeze(1).to_broadcast([128, 16, 32]), op=ALU.add),
                reads=[rk_r[bank], c_r], writes=[c_r])
        k.op(V, lambda: v_.scalar_tensor_tensor(out=val[:], in0=pos[:], scalar=1.0, in1=M_all[:], op0=ALU.add, op1=ALU.mult),
             reads=[c_r, rt_r], writes=[c_r])
        for i in range(NT_OWN):
            k.op(V, lambda i=i: v_.max(out=v8[:, i, :], in_=val[:, i, :]), reads=[c_r], writes=[c_r])
        k.op(V, lambda: v_.tensor_scalar(out=idxf[:], in0=v8[:, :, 0:4], scalar1=-1.0, scalar2=None, op0=ALU.add),
             reads=[c_r], writes=[c_r])
        k.op(V, lambda: v_.tensor_copy(out=idx_i[:], in_=idxf[:]), reads=[c_r], writes=[ix_r])
        for i in range(NT_OWN):
            for kk in range(4):
                k.op(V, lambda i=i, kk=kk: v_.tensor_scalar(out=oh[:], in0=val[:, i, :], scalar1=v8[:, i, kk:kk + 1], scalar2=None,
                                                           op0=ALU.is_equal), reads=[c_r], writes=[c_r])
                k.op(V, lambda i=i, kk=kk: v_.tensor_tensor(out=ohj[:], in0=oh[:], in1=Gd[:, i, :], op=ALU.mult),
                     reads=[c_r, rt_r], writes=[c_r])
                k.op(V, lambda i=i, kk=kk: v_.tensor_reduce(out=gk[:, i, kk:kk + 1], in_=ohj[:], axis=AX.X, op=ALU.add),
                     reads=[c_r], writes=[ix_r])
        k.op(V, lambda: v_.tensor_tensor(out=cmp[:], in0=cw[:, pe_i, :].unsqueeze(1).to_broadcast([128, NBLK, 32]),
                                         in1=bstart[:].unsqueeze(2).to_broadcast([128, NBLK, 32]), op=ALU.is_le),
             reads=[c_r, sm_r], writes=[c_r])
        k.op(V, lambda: v_.tensor_reduce(out=ebf[:], in_=cmp[:], axis=AX.X, op=ALU.add), reads=[c_r], writes=[c_r])
        k.op(V, lambda: v_.tensor_scalar(out=ebf[:], in0=ebf[:], scalar1=31.0, scalar2=None, op0=ALU.min), reads=[c_r], writes=[c_r])
        k.op(V, lambda: v_.tensor_copy(out=ebi[:], in_=ebf[:]), reads=[c_r], writes=[ix_r])
        k.op(V, lambda: v_.scalar_tensor_tensor(out=wf[:], in0=ebf[:].unsqueeze(2).to_broadcast([128, NBLK, 8]), scalar=1024.0,
                                                in1=basepk[:, 0:8].unsqueeze(1).to_broadcast([128, NBLK, 8]), op0=ALU.mult, op1=ALU.add),
             reads=[c_r, sm_r], writes=[c_r])
        k.op(V, lambda: v_.tensor_scalar(out=cmp[:, 0, :], in0=ebf[:, 0:32], scalar1=16.0, scalar2=basepk[:, 8:9],
                                         op0=ALU.mult, op1=ALU.add), reads=[c_r, sm_r], writes=[c_r])
        k.op(V, lambda: v_.tensor_scalar(out=cmp[:, 1, :], in0=ebf[:, 32:64], scalar1=16.0, scalar2=basepk[:, 8:9],
                                         op0=ALU.mult, op1=ALU.add), reads=[c_r, sm_r], writes=[c_r])
        k.op(V, lambda: v_.tensor_copy(out=bidx[:], in_=cmp[:, 0:2, :].rearrange("p a b -> p (a b)")), reads=[c_r], writes=[ix_r])
        k.op(V, lambda: v_.tensor_copy(out=widx[:], in_=wf[:]), reads=[c_r], writes=[ix_r])
        for i in range(NT_OWN):
            k.dma(SP, f"stgld{i % 2}", lambda i=i: nc.sync.dma_start(out=stg[i % 2][:], in_=h2_s[i * 128:(i + 1) * 128, :]),
                  reads=[h2s_r], writes=[stg_r[i % 2]])
            for kk in range(4):
                k.dma(P, f"scat{i % 2}", lambda i=i, kk=kk: g_.indirect_dma_start(
                    out=buf_s[:, :], out_offset=bass.IndirectOffsetOnAxis(ap=idx_i[:, i, kk:kk + 1], axis=0),
                    in_=stg[i % 2][:], in_offset=None), reads=[ix_r, stg_r[i % 2]], writes=[buf_r])
        k.barrier(full=True)
    rt_es.close()

    with ExitStack() as ph:
        wgu = [sb(ph, f"wgu{i}", [128, 8, 2 * D], BF16) for i in range(2)]
        wdn = [sb(ph, f"wdn{i}", [128, 8, D], BF16) for i in range(2)]
        bgr = [sb(ph, f"bgr{i}", [128, 128], F32) for i in range(2)]
        bdr = [sb(ph, f"bdr{i}", [128, D], F32) for i in range(2)]
        bcol = sb(ph, "bcol", [128, 24], F32)
        bcol_r = Res()
        tg = [sb(ph, f"tg{i}", [128, BS], F32) for i in range(2)]
        tg_r = [Res(), Res()]
        w_r = [Res(), Res()]
        xg = [sb(ph, f"xg{i}", [128, 4, D], BF16) for i in range(2)]
        xg_r = [Res(), Res()]
        XT = sb(ph, "XT", [128, 8, BS], BF16)
        XT_r = Res()
        ps_xt = [ps(ph, f"ps_xt{i}", [128, 2 * BS], BF16) for i in range(2)]
        psxt_r = [Res(), Res()]
        ps_g = [ps(ph, f"ps_g{i}", [128, BS], F32) for i in range(2)]
        ps_l = [ps(ph, f"ps_l{i}", [128, BS], F32) for i in range(2)]
        psg_r, psl_r = [Res(), Res()], [Res(), Res()]
        ps_y2 = [ps(ph, f"ps_y2{i}", [128, 512], F32) for i in range(2)]
        psy2_r = [Res(), Res()]
        glu = [sb(ph, f"glu{i}", [128, BS], F32) for i in range(2)]
        sig = [sb(ph, f"sig{i}", [128, BS], F32) for i in range(2)]
        lin = [sb(ph, f"lin{i}", [128, BS], F32) for i in range(2)]
        gl_r, sg_r, ln_r = [Res(), Res()], [Res(), Res()], [Res(), Res()]
        actT = sb(ph, "actT", [128, 8, BS], BF16)
        act_r = Res()
        ysb = [sb(ph, f"ysb{i}", [128, D], F32) for i in range(2)]
        ysb_r = [Res(), Res()]

        def load_block(b):
            p = b % 2
            k.dma(SP, f"xgld{p}", lambda: nc.sync.dma_start(
                out=xg[p][:], in_=buf_s[b * BS:(b + 1) * BS, :].rearrange("(t p) d -> p t d", p=128)),
                reads=[buf_r], writes=[xg_r[p]])
            for kt in range(8):
                k.dma(P, f"wgld{p}", lambda kt=kt: g_.indirect_dma_start(
                    out=wgu[p][:, kt, :], out_offset=None, in_=wgu_b[:, :],
                    in_offset=bass.IndirectOffsetOnAxis(ap=widx[:, b, kt:kt + 1], axis=0)), reads=[ix_r, wb_r], writes=[w_r[p]])
            for kt in range(8):
                k.dma(P, f"wgld{p}", lambda kt=kt: g_.indirect_dma_start(
                    out=wdn[p][:, kt, :], out_offset=None, in_=wd_b[:, :],
                    in_offset=bass.IndirectOffsetOnAxis(ap=widx[:, b, kt:kt + 1], axis=0)), reads=[ix_r, wb_r], writes=[w_r[p]])
            k.dma(P, f"wgld{p}", lambda: g_.indirect_dma_start(
                out=bgr[p][:], out_offset=None, in_=bgu_d[:, :],
                in_offset=bass.IndirectOffsetOnAxis(ap=bidx[:, b:b + 1], axis=0)), reads=[ix_r], writes=[w_r[p]])
            k.dma(P, f"wgld{p}", lambda: g_.indirect_dma_start(
                out=bdr[p][:], out_offset=None, in_=bd_d[:, :],
                in_offset=bass.IndirectOffsetOnAxis(ap=ebi[:, b:b + 1], axis=0)), reads=[ix_r], writes=[w_r[p]])

        load_block(0)
        cg = 0
        cy = 0
        for b in range(NBLK):
            p = b % 2
            if b + 1 < NBLK:
                load_block(b + 1)
            for k2 in range(4):
                bk = k2 % 2
                for kq in range(2):
                    kk = k2 * 2 + kq
                    for tt in range(4):
                        k.op(T, lambda kk=kk, kq=kq, tt=tt, bk=bk: t_.transpose(
                            out=ps_xt[bk][:, kq * BS + tt * 128:kq * BS + (tt + 1) * 128],
                            in_=xg[p][:, tt, kk * 128:(kk + 1) * 128], identity=ident_b[:]),
                            reads=[xg_r[p], cst_r], writes=[psxt_r[bk]], mark=(kq == 1 and tt == 3))
                k.op(A, lambda k2=k2, bk=bk: a_.copy(out=XT[:, k2 * 2:k2 * 2 + 2, :].rearrange("p k s -> p (k s)"),
                                                     in_=ps_xt[bk][:]), reads=[psxt_r[bk]], writes=[XT_r])
            k.op(T, lambda: t_.transpose(out=ps_y2[0][:, 0:128], in_=bgr[p][:], identity=ident_f[:]),
                 reads=[w_r[p], cst_r], writes=[psy2_r[0]])
            k.op(A, lambda: a_.copy(out=bcol[:, 0:16], in_=ps_y2[0][:, 0:16]), reads=[psy2_r[0]], writes=[bcol_r])
            k.op(V, lambda: v_.tensor_scalar(out=bcol[:, 16:24], in0=bcol[:, 8:16], scalar1=1.0, scalar2=None, op0=ALU.add),
                 reads=[bcol_r], writes=[bcol_r])
            for ft in range(8):
                g2 = cg % 2
                cg += 1
                for (pst, pres, coff) in ((ps_g[g2], psg_r[g2], 0), (ps_l[g2], psl_r[g2], D)):
                    for kt in range(8):
                        k.op(T, lambda pst=pst, coff=coff, ft=ft, kt=kt: t_.matmul(
                            pst[:], lhsT=wgu[p][:, kt, coff + ft * 128:coff + (ft + 1) * 128], rhs=XT[:, kt, :],
                            start=(kt == 0), stop=(kt == 7)), reads=[w_r[p], XT_r], writes=[pres], mark=(kt == 7))
                k.op(V, lambda g2=g2, ft=ft: v_.tensor_scalar(out=glu[g2][:], in0=ps_g[g2][:], scalar1=bcol[:, ft:ft + 1], scalar2=7.0,
                                                             op0=ALU.add, op1=ALU.min), reads=[psg_r[g2], bcol_r], writes=[gl_r[g2]])
                k.op(A, lambda g2=g2: a_.activation(out=sig[g2][:], in_=glu[g2][:], func=AF.Sigmoid, scale=1.702),
                     reads=[gl_r[g2]], writes=[sg_r[g2]])
                k.op(V, lambda g2=g2, ft=ft: v_.tensor_scalar(out=lin[g2][:], in0=ps_l[g2][:], scalar1=bcol[:, 16 + ft:17 + ft],
                                                             scalar2=-6.0, op0=ALU.add, op1=ALU.max),
                     reads=[psl_r[g2], bcol_r], writes=[ln_r[g2]])
                k.op(V, lambda g2=g2: v_.tensor_tensor(out=tg[g2][:], in0=glu[g2][:], in1=sig[g2][:], op=ALU.mult),
                     reads=[gl_r[g2], sg_r[g2]], writes=[tg_r[g2]])
                k.op(V, lambda g2=g2, ft=ft: v_.scalar_tensor_tensor(out=actT[:, ft, :], in0=lin[g2][:], scalar=8.0, in1=tg[g2][:],
                                                                    op0=ALU.min, op1=ALU.mult),
                     reads=[ln_r[g2], tg_r[g2]], writes=[act_r])
            for tt in range(4):
                y2 = cy % 2
                cy += 1
                for hf in range(2):
                    for ft in range(8):
                        k.op(T, lambda hf=hf, ft=ft, tt=tt: t_.matmul(ps_y2[hf][:], lhsT=actT[:, ft, tt * 128:(tt + 1) * 128],
                                                                     rhs=wdn[p][:, ft, hf * 512:(hf + 1) * 512],
                                                                     start=(ft == 0), stop=(ft == 7)),
                             reads=[act_r, w_r[p]], writes=[psy2_r[hf]], mark=(ft == 7))
                    k.op(V, lambda hf=hf, y2=y2: v_.tensor_tensor(out=ysb[y2][:, hf * 512:(hf + 1) * 512], in0=ps_y2[hf][:],
                                                                 in1=bdr[p][:, hf * 512:(hf + 1) * 512], op=ALU.add),
                         reads=[psy2_r[hf], w_r[p]], writes=[ysb_r[y2]])
                r0 = b * BS + tt * 128
                k.dma(SP, f"yst{y2}", lambda y2=y2, r0=r0: nc.sync.dma_start(out=ys_s[r0:r0 + 128, :], in_=ysb[y2][:]),
                      reads=[ysb_r[y2]], writes=[ys_r])
        k.barrier()

    with ExitStack() as ph:
        yk = [[sb(ph, f"yk{i}{j}", [128, D], F32) for j in range(4)] for i in range(2)]
        yk_r = [Res(), Res()]
        x1r = [sb(ph, f"x1r{i}", [128, D], F32) for i in range(2)]
        x1r_r = [Res(), Res()]
        acc = [sb(ph, f"acc{i}", [128, D], F32) for i in range(2)]
        acc_r = [Res(), Res()]
        fin_tk = []

        def ldc(i):
            p = i % 2
            for kk in range(4):
                k.dma(P, f"ygld{p}", lambda kk=kk: g_.indirect_dma_start(
                    out=yk[p][kk][:], out_offset=None, in_=ys_s[:, :],
                    in_offset=bass.IndirectOffsetOnAxis(ap=idx_i[:, i, kk:kk + 1], axis=0)), reads=[ix_r, ys_r], writes=[yk_r[p]])
            k.dma(SP, f"x1ld{p}", lambda: nc.sync.dma_start(out=x1r[p][:], in_=out_d[i * 128:(i + 1) * 128, :]),
                  reads=[out_r[i]], writes=[x1r_r[p]])
        ldc(0)
        for i in range(NT_OWN):
            p = i % 2
            if i + 1 < NT_OWN:
                ldc(i + 1)
            k.op(V, lambda: v_.tensor_scalar(out=acc[p][:], in0=yk[p][0][:], scalar1=gk[:, i, 0:1], scalar2=None, op0=ALU.mult),
                 reads=[yk_r[p], ix_r], writes=[acc_r[p]])
            for kk in range(1, 4):
                k.op(V, lambda kk=kk: v_.scalar_tensor_tensor(out=acc[p][:], in0=yk[p][kk][:], scalar=gk[:, i, kk:kk + 1],
                                                             in1=acc[p][:], op0=ALU.mult, op1=ALU.add),
                     reads=[yk_r[p], ix_r, acc_r[p]], writes=[acc_r[p]])
            k.op(V, lambda: v_.tensor_tensor(out=acc[p][:], in0=acc[p][:], in1=bcs[:, 1, :], op=ALU.mult),
                 reads=[acc_r[p], bc_r], writes=[acc_r[p]])
            k.op(V, lambda: v_.tensor_tensor(out=acc[p][:], in0=acc[p][:], in1=x1r[p][:], op=ALU.add),
                 reads=[acc_r[p], x1r_r[p]], writes=[acc_r[p]])
            fin_tk.append(k.dma(SP, f"fin{p}", lambda: nc.sync.dma_start(out=out_d[i * 128:(i + 1) * 128, :], in_=acc[p][:]),
                                reads=[acc_r[p]], writes=[out_r[i]]))
        for t in fin_tk:
            SP.wait(t)
        k.barrier()
    moe_es.close()
    k.es.close()
    return nc


def _rope_tables():
    pos = np.arange(8192)
    row, col = pos // 64, pos % 64
    inv = (1.0 / (10000.0 ** (np.arange(16, dtype=np.float32) / 16))).astype(np.float32)
    ar = row.astype(np.float32)[:, None] * inv
    ac = col.astype(np.float32)[:, None] * inv
    cr, sr, cc, sc = np.cos(ar), np.sin(ar), np.cos(ac), np.sin(ac)
    COS = np.concatenate([cr, cr, cc, cc], 1)
    SIN = np.concatenate([-sr, sr, -sc, sc], 1)
    return np.concatenate([COS, SIN], 1).astype(np.float32)


def _bias_tables(rpb, qh):
    rpb_ext = np.concatenate([rpb.reshape(8, -1), np.full((8, 1), MASKV, np.float32)], 1)
    out = np.empty((5, 128, 6, 8, 128), np.float32)
    slot_i = [0, 1, 2, 30, 31]
    kk = np.arange(128)
    for s, i in enumerate(slot_i):
        j = 32 * qh + i
        win0 = 0 if i == 0 else 30 if i == 31 else i
        qr = 2 * j + kk // 64
        qc = kk % 64
        r0 = np.clip(qr - 4, 0, 120)
        c0 = np.clip(qc - 8, 0, 48)
        for w in range(6):
            g = 32 * qh - 2 + win0 + w
            kr = 2 * g + kk // 64
            kc = kk % 64
            valid = (g >= 0) & (g <= 63)
            inwin = ((kr[:, None] >= r0[None, :]) & (kr[:, None] < r0[None, :] + 8) &
                     (kc[:, None] >= c0[None, :]) & (kc[:, None] < c0[None, :] + 16) & valid)
            ir = kr[:, None] - qr[None, :] + 7
            ic = kc[:, None] - qc[None, :] + 15
            flat = np.where(inwin, np.clip(ir, 0, 14) * 31 + np.clip(ic, 0, 30), 465)
            out[s, :, w, :, :] = rpb_ext[:, flat].transpose(1, 0, 2)
    return out.reshape(5, 128, 6 * 8 * 128)


_NC_CACHE = {}


def kernel(x, c, ctx, c_ctx, w_ada, b_ada, g_attn, w_in, q_norm_diff, k_norm_diff, lam_q1, lam_k1, lam_q2, lam_k2,
           subln_diff, q_norm_na, k_norm_na, rpb_na, out_norm_na, w_out, g_ffn, w_router, b_router, w_gate_up,
           b_gate_up, w_down, b_down):
    f = lambda a: np.ascontiguousarray(np.asarray(a, dtype=np.float32))
    x, c, ctx, c_ctx = f(x), f(c), f(ctx), f(c_ctx)
    if "nc" not in _NC_CACHE:
        _NC_CACHE["nc"] = build()
    nc = _NC_CACHE["nc"]
    rope_t = _rope_tables()
    colT = lambda v: np.ascontiguousarray(v.reshape(-1, 128).T)
    shared = {
        "badaT": colT(f(b_ada)[0]),
        "gT": np.concatenate([colT(f(g_attn)[0]), colT(f(g_ffn)[0])], 1),
        "gains": np.concatenate([np.tile(f(q_norm_diff)[0], 8), np.tile(f(k_norm_diff)[0], 8), np.tile(f(q_norm_na)[0], 8),
                                 np.tile(f(k_norm_na)[0], 8), f(out_norm_na)[0]])[None, :],
        "lamv": np.concatenate([f(lam_q1)[0], f(lam_k1)[0], f(lam_q2)[0], f(lam_k2)[0]])[None, :],
        "sublnT": f(subln_diff)[0].reshape(128, 1),
        "w_ada": f(w_ada)[0], "w_in": f(w_in)[0], "w_out": f(w_out)[0],
        "w_routerT": np.ascontiguousarray(f(w_router)[0].reshape(8, 128, 32).transpose(1, 0, 2).reshape(128, 256)),
        "b_router": f(b_router)[0][None, :],
        "w_gate_up": f(w_gate_up)[0].reshape(32 * D, 2 * D), "w_down": f(w_down)[0].reshape(32 * D, D),
        "b_gate_up": f(b_gate_up)[0].reshape(32 * 16, 128), "b_down": f(b_down)[0],
        "bstart": (np.arange(NBLK, dtype=np.float32) * BS)[None, :],
        "basepk": np.concatenate([np.arange(8)[None, :] * 128 + np.arange(128)[:, None],
                                  (np.arange(128) % 16)[:, None]], 1).astype(np.float32),
    }
    rpb = f(rpb_na)[0]
    bias_q = [_bias_tables(rpb, 0), _bias_tables(rpb, 1)]
    in_maps = []
    for core in range(NCORES):
        b, qh = core // 2, core % 2
        xb = x[b]
        own = xb[qh * 4096:(qh + 1) * 4096]
        oth = xb[(1 - qh) * 4096:(2 - qh) * 4096]
        halo = np.zeros((4 * 128, D), np.float32)
        for n, g in enumerate([32 * qh - 2, 32 * qh - 1, 32 * qh + 32, 32 * qh + 33]):
            if 0 <= g <= 63:
                halo[n * 128:(n + 1) * 128] = xb[g * 128:(g + 1) * 128]
        x_all = np.concatenate([own, oth, ctx[b], halo], 0)
        rp = np.concatenate([rope_t[qh * 4096:(qh + 1) * 4096], rope_t[(1 - qh) * 4096:(2 - qh) * 4096]], 0)
        cv = np.stack([c[b], c_ctx], 1).reshape(8, 128, 2).transpose(1, 0, 2).reshape(128, 16)
        m = dict(shared)
        m.update({"x_all": x_all, "rope": np.ascontiguousarray(rp), "cT": np.ascontiguousarray(cv), "biasm": bias_q[qh]})
        in_maps.append(m)
    res = run_bass_kernel_spmd(nc, in_maps, core_ids=list(range(NCORES)))
    out = np.empty((4, 8192, D), np.float32)
    for core in range(NCORES):
        b, qh = core // 2, core % 2
        out[b, qh * 4096:(qh + 1) * 4096] = res.results[core]["out"]
    return out
```
